# Optimizing a Trainium2 kernel written in Bass

```python
import math
import jax
import jax.numpy as jnp
from jax import lax
import numpy as np

D_MODEL = 1024
BATCH = 32
SEQ = 2048
DEPTH = 4

GRID_W = 64
CTX_LEN = 256
N_MIXERS = 2

DA_HEADS = 8
DA_HEAD_DIM = 64
DA_V_DIM = 2 * DA_HEAD_DIM
DA_QK = DA_HEADS * 2 * DA_HEAD_DIM
DA_SCALE = DA_HEAD_DIM ** -0.5
Q_BLOCK = 128

RT_HEADS = 4
RT_QK_DIM = 256
RT_V_DIM = 512
RT_QK = RT_HEADS * RT_QK_DIM
RT_V = RT_HEADS * RT_V_DIM
RT_IN = 2 * RT_QK + 3 * RT_V
RT_SPLITS = (RT_QK, 2 * RT_QK, 2 * RT_QK + RT_V, 2 * RT_QK + 2 * RT_V)
RT_SCALE = RT_QK_DIM ** -0.5
RT_CHUNK = 128

MOE_GROUPS = 4
MOE_EXPERTS_PER_GROUP = 8
MOE_EXPERTS = MOE_GROUPS * MOE_EXPERTS_PER_GROUP
MOE_TOP_K = 2
MOE_HIDDEN = 512

ROPE_BASE = 10000.0
RMS_EPS = 1e-6

kernel_name = "hybrid_diffattn_retention_hmoe_dit"


def _rms_normalize(x):
    xf = x.astype(jnp.float32)
    return xf * lax.rsqrt(jnp.mean(xf * xf, axis=-1, keepdims=True) + RMS_EPS)


def _rms_norm(x, g):
    return (_rms_normalize(x) * g.astype(jnp.float32)).astype(x.dtype)


def _modulate(h, shift, scale):
    return h * (1.0 + scale) + shift


def _axial_rope(n_tokens, head_dim):
    rows = n_tokens // GRID_W
    t = jnp.arange(rows * GRID_W)
    row = (t // GRID_W).astype(jnp.float32)
    col = (t % GRID_W).astype(jnp.float32)
    n_freq = head_dim // 4
    inv_freq = ROPE_BASE ** (-jnp.arange(n_freq, dtype=jnp.float32) / n_freq)
    ang = jnp.concatenate([row[:, None] * inv_freq, col[:, None] * inv_freq], axis=-1)
    return jnp.cos(ang), jnp.sin(ang)


def _apply_rope(x, cos, sin):
    extra = x.ndim - 3
    c = cos.reshape(cos.shape[0], *([1] * extra), cos.shape[1])
    s = sin.reshape(sin.shape[0], *([1] * extra), sin.shape[1])
    x1, x2 = jnp.split(x.astype(jnp.float32), 2, axis=-1)
    return jnp.concatenate([x1 * c - x2 * s, x1 * s + x2 * c], axis=-1).astype(x.dtype)


def _diff_attention(h, n_ctx, w_qkv, q_g, k_g, lam, subln_g, w_o, lam_init, cos, sin, need_ctx):
    B, N, _ = h.shape
    S = N - n_ctx
    q, k, v = jnp.split(h @ w_qkv, 3, axis=-1)
    q = _rms_norm(q.reshape(B, N, DA_HEADS, 2, DA_HEAD_DIM), q_g)
    k = _rms_norm(k.reshape(B, N, DA_HEADS, 2, DA_HEAD_DIM), k_g)
    v = v.reshape(B, N, DA_HEADS, DA_V_DIM)
    q_lat = _apply_rope(q[:, n_ctx:], cos, sin)
    k = jnp.concatenate([k[:, :n_ctx], _apply_rope(k[:, n_ctx:], cos, sin)], axis=1)
    lam_f = lam.astype(jnp.float32)
    lam_full = jnp.exp(jnp.sum(lam_f[0] * lam_f[1])) - jnp.exp(jnp.sum(lam_f[2] * lam_f[3])) + lam_init

    def attend(qb, kb, vb):
        s = jnp.einsum('bqhmd,bkhmd->bhmqk', qb, kb).astype(jnp.float32) * DA_SCALE
        p = jax.nn.softmax(s, axis=-1)
        a = p[:, :, 0] - lam_full * p[:, :, 1]
        return jnp.einsum('bhqk,bkhe->bqhe', a.astype(vb.dtype), vb)

    nb = S // Q_BLOCK
    q_blocks = jnp.moveaxis(q_lat.reshape(B, nb, Q_BLOCK, DA_HEADS, 2, DA_HEAD_DIM), 1, 0)
    o_lat = lax.map(lambda qb: attend(qb, k, v), q_blocks)
    o_lat = jnp.moveaxis(o_lat, 0, 1).reshape(B, S, DA_HEADS, DA_V_DIM)
    if need_ctx:
        o_ctx = attend(q[:, :n_ctx], k[:, :n_ctx], v[:, :n_ctx])
        o = jnp.concatenate([o_ctx, o_lat], axis=1)
    else:
        o = o_lat
    o = _rms_norm(o, subln_g) * (1.0 - lam_init)
    return o.reshape(B, o.shape[1], DA_HEADS * DA_V_DIM) @ w_o


def _retention_scan(q, k, v, log_g, state0):
    B, H, n, dk = q.shape
    dv = v.shape[-1]
    nc = n // RT_CHUNK
    pos = jnp.arange(RT_CHUNK, dtype=jnp.float32)
    rel = pos[:, None] - pos[None, :]
    lg = log_g[:, None]
    decay_intra = jnp.where(rel >= 0, jnp.exp(log_g[:, None, None] * jnp.maximum(rel, 0.0)), 0.0)
    decay_q = jnp.exp(lg * (pos + 1.0))[:, :, None]
    decay_k = jnp.exp(lg * (RT_CHUNK - 1.0 - pos))[:, :, None]
    decay_chunk = jnp.exp(log_g * RT_CHUNK)[:, None, None]

    def chunks(t):
        return jnp.moveaxis(t.astype(jnp.float32).reshape(B, H, nc, RT_CHUNK, t.shape[-1]), 2, 0)

    def step(state, qkv):
        qc, kc, vc = qkv
        scores = jnp.einsum('bhqd,bhkd->bhqk', qc, kc) * decay_intra
        y = jnp.einsum('bhqk,bhke->bhqe', scores, vc) + jnp.einsum('bhqd,bhde->bhqe', qc, state) * decay_q
        state = state * decay_chunk + jnp.einsum('bhkd,bhke->bhde', kc * decay_k, vc)
        return state, y

    state, ys = lax.scan(step, state0, (chunks(q), chunks(k), chunks(v)))
    return jnp.moveaxis(ys, 0, 2).reshape(B, H, n, dv), state


def _final_state(k, v, log_g):
    n = k.shape[2]
    w = jnp.exp(log_g[:, None] * (n - 1.0 - jnp.arange(n, dtype=jnp.float32)))
    return jnp.einsum('bhnd,hn,bhne->bhde', k.astype(jnp.float32), w, v.astype(jnp.float32))


def _retention_direction(q, k, v, n_ctx, log_g, need_ctx):
    qc, kc, vc = q[:, :, :n_ctx], k[:, :, :n_ctx], v[:, :, :n_ctx]
    if need_ctx:
        zero = jnp.zeros((q.shape[0], RT_HEADS, RT_QK_DIM, RT_V_DIM), jnp.float32)
        y_ctx, s_ctx = _retention_scan(qc, kc, vc, log_g, zero)
    else:
        y_ctx, s_ctx = None, _final_state(kc, vc, log_g)
    y_lat, _ = _retention_scan(q[:, :, n_ctx:], k[:, :, n_ctx:], v[:, :, n_ctx:], log_g, s_ctx)
    return y_ctx, y_lat


def _flip_parts(t, n_ctx):
    return jnp.concatenate([jnp.flip(t[:, :, :n_ctx], axis=2), jnp.flip(t[:, :, n_ctx:], axis=2)], axis=2)


def _retention(h, n_ctx, w_in, decay, w_o, cos, sin, need_ctx):
    B, N, _ = h.shape
    q, k, v, g_f, g_b = jnp.split(h @ w_in, RT_SPLITS, axis=-1)

    def rope_lat(t):
        return jnp.concatenate([t[:, :n_ctx], _apply_rope(t[:, n_ctx:], cos, sin)], axis=1)

    q = jnp.transpose(rope_lat(q.reshape(B, N, RT_HEADS, RT_QK_DIM)), (0, 2, 1, 3))
    k = jnp.transpose(rope_lat(k.reshape(B, N, RT_HEADS, RT_QK_DIM)) * RT_SCALE, (0, 2, 1, 3))
    v = jnp.transpose(v.reshape(B, N, RT_HEADS, RT_V_DIM), (0, 2, 1, 3))
    log_g = jnp.log1p(-jnp.exp(decay.astype(jnp.float32)))
    yc_f, yl_f = _retention_direction(q, k, v, n_ctx, log_g[0], need_ctx)
    yc_b, yl_b = _retention_direction(_flip_parts(q, n_ctx), _flip_parts(k, n_ctx), _flip_parts(v, n_ctx),
                                      n_ctx, log_g[1], need_ctx)
    yl_b = jnp.flip(yl_b, axis=2)
    if need_ctx:
        y_f = jnp.concatenate([yc_f, yl_f], axis=2)
        y_b = jnp.concatenate([jnp.flip(yc_b, axis=2), yl_b], axis=2)
        gf, gb = g_f, g_b
    else:
        y_f, y_b = yl_f, yl_b
        gf, gb = g_f[:, n_ctx:], g_b[:, n_ctx:]

    def group_norm(y):
        y = jnp.transpose(y, (0, 2, 1, 3))
        return _rms_normalize(y).reshape(B, y.shape[1], RT_V)

    o = group_norm(y_f) * jax.nn.silu(gf.astype(jnp.float32)) + group_norm(y_b) * jax.nn.silu(gb.astype(jnp.float32))
    return o.astype(h.dtype) @ w_o


def _hier_moe(h, w_group, b_group, w_expert, b_expert, w_gate, w_up, w_down):
    B, N, D = h.shape
    t = h.reshape(B * N, D)
    p_group = jax.nn.softmax((t @ w_group + b_group).astype(jnp.float32), axis=-1)
    p_sel, g_idx = lax.top_k(p_group, 1)
    e_logits = (t @ w_expert + b_expert).astype(jnp.float32).reshape(B * N, MOE_GROUPS, MOE_EXPERTS_PER_GROUP)
    e_logits = jnp.einsum('tge,tg->te', e_logits, jax.nn.one_hot(g_idx[:, 0], MOE_GROUPS, dtype=jnp.float32))
    top_logit, e_idx = lax.top_k(e_logits, MOE_TOP_K)
    w_sel = jax.nn.softmax(top_logit, axis=-1) * p_sel
    expert_id = g_idx * MOE_EXPERTS_PER_GROUP + e_idx
    combine = jnp.einsum('tk,tke->te', w_sel,
                         jax.nn.one_hot(expert_id, MOE_EXPERTS, dtype=jnp.float32)).astype(h.dtype)
    y = jnp.zeros_like(t)
    for e in range(MOE_EXPERTS):
        a = jax.nn.silu(t @ w_gate[e]) * (t @ w_up[e])
        y = y + combine[:, e:e + 1] * (a @ w_down[e])
    return y.reshape(B, N, D)


def setup_inputs(seed: int = 0) -> dict:
    key = jax.random.key(seed)
    ks = jax.random.split(key, 24)
    D = D_MODEL
    n_da = (DEPTH + N_MIXERS - 1) // N_MIXERS
    n_rt = DEPTH // N_MIXERS

    def nrm(k, shape, s):
        return jax.random.normal(k, shape, jnp.float32) * s

    rt_decay = (-(5.0 + jnp.arange(RT_HEADS, dtype=jnp.float32)) * math.log(2.0)
                + nrm(ks[15], (n_rt, 2, RT_HEADS), 0.05))
    return {
        "x": nrm(ks[0], (BATCH, SEQ, D), 1.0),
        "c": nrm(ks[1], (BATCH, D), 1.0),
        "ctx": nrm(ks[2], (BATCH, CTX_LEN, D), 1.0),
        "c_ctx": nrm(ks[3], (D,), 1.0),
        "norm1_g": 1.0 + nrm(ks[4], (DEPTH, D), 0.02),
        "norm2_g": 1.0 + nrm(ks[5], (DEPTH, D), 0.02),
        "ada_w": nrm(ks[6], (DEPTH, D, 6 * D), 0.5 * D ** -0.5),
        "ada_b": nrm(ks[7], (DEPTH, 6 * D), 0.02),
        "da_w_qkv": nrm(ks[8], (n_da, D, 3 * DA_QK), D ** -0.5),
        "da_q_norm_g": 1.0 + nrm(ks[9], (n_da, DA_HEAD_DIM), 0.02),
        "da_k_norm_g": 1.0 + nrm(ks[10], (n_da, DA_HEAD_DIM), 0.02),
        "da_lambda": nrm(ks[11], (n_da, 4, DA_HEAD_DIM), 0.1),
        "da_subln_g": 1.0 + nrm(ks[12], (n_da, DA_V_DIM), 0.02),
        "da_w_o": nrm(ks[13], (n_da, DA_HEADS * DA_V_DIM, D), (DA_HEADS * DA_V_DIM) ** -0.5),
        "rt_w_in": nrm(ks[14], (n_rt, D, RT_IN), D ** -0.5),
        "rt_decay": rt_decay,
        "rt_w_o": nrm(ks[16], (n_rt, RT_V, D), RT_V ** -0.5),
        "moe_w_group": nrm(ks[17], (DEPTH, D, MOE_GROUPS), D ** -0.5),
        "moe_b_group": nrm(ks[18], (DEPTH, MOE_GROUPS), 0.01),
        "moe_w_expert": nrm(ks[19], (DEPTH, D, MOE_EXPERTS), D ** -0.5),
        "moe_b_expert": nrm(ks[20], (DEPTH, MOE_EXPERTS), 0.01),
        "moe_w_gate": nrm(ks[21], (DEPTH, MOE_EXPERTS, D, MOE_HIDDEN), D ** -0.5),
        "moe_w_up": nrm(ks[22], (DEPTH, MOE_EXPERTS, D, MOE_HIDDEN), D ** -0.5),
        "moe_w_down": nrm(ks[23], (DEPTH, MOE_EXPERTS, MOE_HIDDEN, D), MOE_HIDDEN ** -0.5),
    }


def reference(x, c, ctx, c_ctx, norm1_g, norm2_g, ada_w, ada_b, da_w_qkv, da_q_norm_g, da_k_norm_g,
              da_lambda, da_subln_g, da_w_o, rt_w_in, rt_decay, rt_w_o, moe_w_group, moe_b_group,
              moe_w_expert, moe_b_expert, moe_w_gate, moe_w_up, moe_w_down):
    S = x.shape[1]
    n_ctx = ctx.shape[1]
    cos_da, sin_da = _axial_rope(S, DA_HEAD_DIM)
    cos_rt, sin_rt = _axial_rope(S, RT_QK_DIM)
    silu_c = jax.nn.silu(c)[:, None, :]
    silu_cc = jax.nn.silu(c_ctx)[None, None, :]
    h_lat, h_ctx = x, ctx
    for i in range(DEPTH):
        need_ctx = i < DEPTH - 1
        m_lat = jnp.split(silu_c @ ada_w[i] + ada_b[i], 6, axis=-1)
        m_ctx = jnp.split(silu_cc @ ada_w[i] + ada_b[i], 6, axis=-1)

        a = jnp.concatenate([_modulate(_rms_norm(h_ctx, norm1_g[i]), m_ctx[0], m_ctx[1]),
                             _modulate(_rms_norm(h_lat, norm1_g[i]), m_lat[0], m_lat[1])], axis=1)
        j = i // N_MIXERS
        if i % N_MIXERS == 0:
            lam_init = 0.8 - 0.6 * math.exp(-0.3 * i)
            o = _diff_attention(a, n_ctx, da_w_qkv[j], da_q_norm_g[j], da_k_norm_g[j], da_lambda[j],
                                da_subln_g[j], da_w_o[j], lam_init, cos_da, sin_da, need_ctx)
        else:
            o = _retention(a, n_ctx, rt_w_in[j], rt_decay[j], rt_w_o[j], cos_rt, sin_rt, need_ctx)
        h_lat = h_lat + m_lat[2] * o[:, -S:]

        moe = (moe_w_group[i], moe_b_group[i], moe_w_expert[i], moe_b_expert[i],
               moe_w_gate[i], moe_w_up[i], moe_w_down[i])
        f_lat_in = _modulate(_rms_norm(h_lat, norm2_g[i]), m_lat[3], m_lat[4])
        if need_ctx:
            h_ctx = h_ctx + m_ctx[2] * o[:, :n_ctx]
            f_ctx_in = _modulate(_rms_norm(h_ctx, norm2_g[i]), m_ctx[3], m_ctx[4])
            f = _hier_moe(jnp.concatenate([f_ctx_in, f_lat_in], axis=1), *moe)
            h_ctx = h_ctx + m_ctx[5] * f[:, :n_ctx]
            h_lat = h_lat + m_lat[5] * f[:, n_ctx:]
        else:
            f = _hier_moe(f_lat_in, *moe)
            h_lat = h_lat + m_lat[5] * f
    return h_lat
```

```python
import math
import numpy as np
import concourse.bass as bass
import concourse.mybir as mybir
from concourse.bass_utils import run_bass_kernel_spmd

F32 = mybir.dt.float32
BF = mybir.dt.bfloat16
I32 = mybir.dt.int32
AF = mybir.ActivationFunctionType
ALU = mybir.AluOpType
AX = mybir.AxisListType

D = 1024
S = 2048
L = 256
N = S + L
NST = N // 128
DEPTH = 4
NE = 32
HID = 512
EPS = 1e-6


def _esize(dt):
    return 2 if dt == BF else 4


class Prog:
    def __init__(self, nc, readonly):
        self.nc = nc
        self.ops = []
        self.readonly = set(readonly)
        self.state = {}

    def add(self, eng, fn, ins=(), outs=(), dma=False):
        self.ops.append([eng, fn, list(ins), list(outs), dma])

    def _grans(self, ap):
        name = ap.tensor.name
        if name in self.readonly:
            return name, ()
        es = _esize(ap.dtype)
        dims = [list(d) for d in ap.ap]
        sp = str(ap.space) if hasattr(ap, "space") else ""
        onchip = ("SB" in sp.upper()) or ("PSUM" in sp.upper()) or ("STATE" in sp.upper())
        if name.startswith("ps"):
            return name, (0,)
        if onchip:
            pstride = dims[0][0]
            off = ap.offset % pstride if pstride else ap.offset
            free = dims[1:]
            G = 512
        else:
            off = ap.offset
            free = dims
            G = 65536
        free = [d for d in free if d[1] > 1]
        if not free:
            lo = off * es
            return name, tuple(range(lo // G, (lo + es - 1) // G + 1))
        inner = free[-1]
        outer = free[:-1]
        nint = 1
        for d in outer:
            nint *= d[1]
        span_in = (inner[1] - 1) * abs(inner[0]) + 1
        res = set()
        if nint > 128:
            hi = off + sum((d[1] - 1) * abs(d[0]) for d in free) + 1
            return name, tuple(range(off * es // G, (hi * es - 1) // G + 1))
        idx = [0] * len(outer)
        while True:
            st = off + sum(i * d[0] for i, d in zip(idx, outer))
            res.update(range(st * es // G, ((st + span_in) * es - 1) // G + 1))
            k = len(outer) - 1
            while k >= 0:
                idx[k] += 1
                if idx[k] < outer[k][1]:
                    break
                idx[k] = 0
                k -= 1
            if k < 0:
                break
        return name, tuple(res)

    def finalize(self, out_names):
        nc = self.nc
        ops = self.ops
        n = len(ops)
        deps = [None] * n
        needed = [False] * n
        state = {}
        for i, (eng, fn, ins, outs, dma) in enumerate(ops):
            dset = set()
            rg = [self._grans(a) for a in ins]
            wg = [self._grans(a) for a in outs]
            for name, gs in rg:
                for g in gs:
                    st = state.get((name, g))
                    if st is not None and st[0] is not None:
                        dset.add(st[0])
            for name, gs in wg:
                for g in gs:
                    st = state.get((name, g))
                    if st is not None:
                        if st[0] is not None:
                            dset.add(st[0])
                        dset.update(st[1].values())
            rkey = eng if (not dma and eng in ("pe", "act", "dve")) else i
            for name, gs in rg:
                for g in gs:
                    st = state.get((name, g))
                    if st is None:
                        state[(name, g)] = [None, {rkey: i}]
                    else:
                        st[1][rkey] = i
            for name, gs in wg:
                for g in gs:
                    state[(name, g)] = [i, {}]
            dset.discard(i)
            dl = []
            for d in dset:
                if ops[d][0] == "pe" and eng == "pe" and not ops[d][4] and not dma:
                    continue
                dl.append(d)
                needed[d] = True
            deps[i] = dl
        streams = ["pe", "act", "dve", "pool", "sp"]
        NDS = 8
        import contextlib
        self._stack = contextlib.ExitStack()
        mk = lambda nm: self._stack.enter_context(nc.semaphore(nm))
        esem = {s: [mk(f"e_{s}_0")] for s in streams}
        ecount = {s: 0 for s in streams}
        dsem = {s: [mk(f"d_{s}_{k}") for k in range(NDS)] for s in ("sp", "pool", "act")}
        dcount = {s: [0] * NDS for s in dsem}
        dnext = {s: 0 for s in dsem}
        signal = [None] * n
        prewait = [None] * n
        for i, (eng, fn, ins, outs, dma) in enumerate(ops):
            if dma:
                k = dnext[eng]
                dnext[eng] = (k + 1) % NDS
                if dcount[eng][k] > 0:
                    prewait[i] = (dsem[eng][k], dcount[eng][k])
                dcount[eng][k] += 16
                signal[i] = (dsem[eng][k], dcount[eng][k], 16)
            elif needed[i]:
                if ecount[eng] >= 30000:
                    esem[eng].append(mk(f"e_{eng}_{len(esem[eng])}"))
                    ecount[eng] = 0
                ecount[eng] += 1
                signal[i] = (esem[eng][-1], ecount[eng], 1)
        known = {s: {} for s in streams}
        waits = [None] * n
        for i, (eng, fn, ins, outs, dma) in enumerate(ops):
            w = {}
            kn = known[eng]
            if prewait[i] is not None:
                sem, val = prewait[i]
                if kn.get(sem.num, 0) < val:
                    w[sem.num] = (sem, val)
            for d in deps[i]:
                sem, val, _ = signal[d]
                if kn.get(sem.num, 0) < val and w.get(sem.num, (None, 0))[1] < val:
                    w[sem.num] = (sem, val)
            for k, (sem, val) in w.items():
                kn[k] = val
            waits[i] = list(w.values())
        final = []
        for s in dsem:
            for k in range(NDS):
                if dcount[s][k] > 0:
                    final.append((dsem[s][k], dcount[s][k]))
        by = {s: [i for i in range(n) if ops[i][0] == s] for s in streams}
        self.n_ops = n
        finalcount = {}
        for i in range(n):
            if signal[i] is not None:
                sem, val, _ = signal[i]
                finalcount[sem.num] = max(finalcount.get(sem.num, 0), val)
        bad = []
        for i in range(n):
            for sem, val in waits[i]:
                if val > finalcount.get(sem.num, 0):
                    bad.append((i, ops[i][0], sem.num, val, finalcount.get(sem.num, 0)))
        for i in range(n):
            for d in deps[i]:
                if signal[d] is None:
                    bad.append((i, ops[i][0], "dep-without-signal", d, ops[d][0]))
        nd = sum(1 for o in ops if o[4])
        print(f"[prog] ops={n} dma={nd} waits={sum(len(w) for w in waits)} "
              f"signals={sum(1 for x in signal if x is not None)} unreachable={len(bad)}", flush=True)
        if bad:
            raise RuntimeError(f"unreachable semaphore waits (first 10): {bad[:10]}")

        def emit(stream, e):
            for i in by[stream]:
                for sem, val in waits[i]:
                    e.wait_ge(sem, val)
                ins = ops[i][1](e)
                if signal[i] is not None:
                    ins.then_inc(signal[i][0], signal[i][2])
            if stream == "sp":
                for sem, val in final:
                    e.wait_ge(sem, val)

        with nc.Block() as block:
            @block.tensor
            def _(e):
                emit("pe", e)

            @block.scalar
            def _(e):
                emit("act", e)

            @block.vector
            def _(e):
                emit("dve", e)

            @block.gpsimd
            def _(e):
                emit("pool", e)

            @block.sync
            def _(e):
                emit("sp", e)
        self._stack.close()


def build_program(nb, layers, consts_shape, stop_after=None):
    nc = bass.Bass("TRN2", target_bir_lowering=False)
    dt = nc.dram_tensor
    ext = {}

    def EI(name, shape, dtype=F32):
        ext[name] = dt(name, list(shape), dtype, kind="ExternalInput").ap()
        return ext[name]

    x = EI("x", [nb, S, D])
    ctx = EI("ctx", [nb, L, D])
    cT = EI("cT", [128, 8, nb + 1])
    consts = EI("consts", consts_shape)
    rope = EI("rope", [4, 128, S])
    ada_bT = EI("ada_bT", [DEPTH, 128, 48])
    normgT = EI("normgT", [2, DEPTH, 128, 8])
    norm1_g = EI("norm1_g", [DEPTH, D])
    norm2_g = EI("norm2_g", [DEPTH, D])
    ada_w = EI("ada_w", [DEPTH, D, 6 * D])
    ada_b = EI("ada_b", [DEPTH, 6 * D])
    da_w_qkv = EI("da_w_qkv", [2, D, 3 * D])
    da_q_norm_g = EI("da_q_norm_g", [2, 64])
    da_k_norm_g = EI("da_k_norm_g", [2, 64])
    da_lambda = EI("da_lambda", [2, 4, 64])
    da_subln_g = EI("da_subln_g", [2, 128])
    da_w_o = EI("da_w_o", [2, D, D])
    rt_w_in = EI("rt_w_in", [2, D, 8192])
    rt_decay = EI("rt_decay", [2, 2, 4])
    rt_w_o = EI("rt_w_o", [2, 2048, D])
    moe_w_group = EI("moe_w_group", [DEPTH, D, 4])
    moe_b_group = EI("moe_b_group", [DEPTH, 4])
    moe_w_expert = EI("moe_w_expert", [DEPTH, D, 32])
    moe_b_expert = EI("moe_b_expert", [DEPTH, 32])
    moe_w_gate = EI("moe_w_gate", [DEPTH, NE, D, HID])
    moe_w_up = EI("moe_w_up", [DEPTH, NE, D, HID])
    moe_w_down = EI("moe_w_down", [DEPTH, NE, HID, D])
    out = dt("out", [nb, S, D], F32, kind="ExternalOutput").ap()
    hbuf = dt("hbuf", [nb, N, D], F32, kind="Internal").ap()
    gsc = dt("gsc", [DEPTH, 2, nb + 1, D], F32, kind="Internal").ap()
    obuf = dt("obuf", [N, 2048], BF, kind="Internal").ap()

    P = Prog(nc, readonly=list(ext.keys()))
    import contextlib
    stack = contextlib.ExitStack()

    def sb(name, shape, dtype):
        return stack.enter_context(nc.sbuf_tensor(name, list(shape), dtype))

    def psum(name):
        return stack.enter_context(nc.psum_tensor(name, [128, 512], F32))

    PS = [psum(f"ps{i}") for i in range(8)]
    ps_rr = [0]

    def dma(q, out_ap, in_ap):
        P.add(q, lambda e: e.dma_start(out=out_ap, in_=in_ap), [in_ap], [out_ap], dma=True)

    def mm(out_ap, lhsT, rhs, start, stop):
        P.add("pe", lambda e: e.matmul(out_ap, lhsT, rhs, start=start, stop=stop), [lhsT, rhs], [out_ap])

    def tr(out_ap, in_ap, ident):
        P.add("pe", lambda e: e.transpose(out_ap, in_ap, ident), [in_ap, ident], [out_ap])

    def act(out_ap, in_ap, func, bias=None, scale=None, accum=None):
        kw = {}
        ins = [in_ap]
        outs = [out_ap]
        if bias is not None:
            kw["bias"] = bias
            if not isinstance(bias, float):
                ins.append(bias)
        if scale is not None:
            kw["scale"] = scale
            if not isinstance(scale, float):
                ins.append(scale)
        if accum is not None:
            kw["accum_out"] = accum
            outs.append(accum)
        P.add("act", lambda e: e.activation(out_ap, in_ap, func, **kw), ins, outs)

    def tt(eng, out_ap, a, b, op):
        P.add(eng, lambda e: e.tensor_tensor(out_ap, a, b, op), [a, b], [out_ap])

    def ts(eng, out_ap, a, s1, s2, op0, op1=None):
        ins = [a] + [s for s in (s1, s2) if s is not None and not isinstance(s, (float, int))]
        if op1 is None:
            P.add(eng, lambda e: e.tensor_scalar(out_ap, a, s1, None, op0), ins, [out_ap])
        else:
            P.add(eng, lambda e: e.tensor_scalar(out_ap, a, s1, s2, op0, op1), ins, [out_ap])

    def stt(out_ap, a, s, b, op0, op1):
        ins = [a, b] + ([] if isinstance(s, (float, int)) else [s])
        P.add("dve", lambda e: e.scalar_tensor_tensor(out_ap, a, s, b, op0, op1), ins, [out_ap])

    def cp(eng, out_ap, in_ap):
        if eng == "act":
            P.add("act", lambda e: e.copy(out_ap, in_ap), [in_ap], [out_ap])
        else:
            P.add(eng, lambda e: e.tensor_copy(out_ap, in_ap), [in_ap], [out_ap])

    def recip(out_ap, in_ap):
        P.add("dve", lambda e: e.reciprocal(out_ap, in_ap), [in_ap], [out_ap])

    def memset(eng, ap, v):
        P.add(eng, lambda e: e.memset(ap, v), [], [ap])

    def nextps():
        p = PS[ps_rr[0] % 8]
        ps_rr[0] += 1
        return p

    CO = consts_offsets()
    cst = sb("cst", [128, CO["_w"]], F32)
    dma("sp", cst[:, :], consts[:, :])
    ident_b = sb("ident_b", [128, 128], BF)
    rot_b = sb("rot_b", [128, 128], BF)
    ones_b = sb("ones_b", [128, 128], BF)

    def C(name, w):
        o = CO[name]
        return cst[:, o:o + w]

    cp("dve", ident_b[:, :], C("ident", 128))
    cp("dve", rot_b[:, :], C("rot", 128))
    memset("dve", ones_b[:, :], 1.0)
    def rope_tile(idx, c0, tn):
        w = nwk()
        dma("sp", w[:, 0:tn], rope[idx, :, c0:c0 + tn])
        return w[:, 0:tn]
    blk64 = C("blk64", 128)
    ones128 = C("ones128", 128)
    ones1 = C("ones1", 128)
    pos = C("pos", 1)
    rpos = C("rpos", 1)
    relT = C("relT", 128)
    ident_f = C("ident", 128)

    ARA = sb("ARA", [128, 8 * N], BF)
    ARB = sb("ARB", [128, 8 * N], BF)
    ARC = sb("ARC", [128, 6 * N], BF)
    WS = [sb(f"WS{i}", [128, 4096], BF) for i in range(3)]
    WSX = [ARA[:, i * 4096:(i + 1) * 4096] for i in range(4)]
    ws_rr = [0]
    ws_pool = [WS]

    def wslot():
        pool = ws_pool[0]
        w = pool[ws_rr[0] % len(pool)]
        ws_rr[0] += 1
        return w

    aT = ARA[:, :].rearrange("p (c t) -> p c t", c=8)
    xt = [sb(f"xt{i}", [128, D], F32) for i in range(3)]
    xt_rr = [0]
    tmpf = [sb(f"tmpf{i}", [128, D], F32) for i in range(1)]
    abf = [sb(f"abf{i}", [128, D], BF) for i in range(2)]
    small = sb("small", [128, 64], F32)
    Gt = [sb(f"Gt{s}", [128, D], F32) for s in range(2)]
    ABt = sb("ABt", [128, 2, 2, 8], F32)
    wk = [sb(f"wk{i}", [128, 512], F32) for i in range(8)]
    wk_rr = [0]
    wkb = [sb(f"wkb{i}", [128, 512], BF) for i in range(4)]
    wkb_rr = [0]

    def nwk():
        w = wk[wk_rr[0] % len(wk)]
        wk_rr[0] += 1
        return w

    def nwkb():
        w = wkb[wkb_rr[0] % len(wkb)]
        wkb_rr[0] += 1
        return w

    ns = nb + 1
    scT = sb("scT", [128, 8, ns], BF)
    cTs = sb("cTs", [128, 8, ns], F32)
    dma("sp", cTs[:, :, :], cT[:, :, :])
    act(scT[:, :, :], cTs[:, :, :], AF.Silu)
    screp = ARB[:, 0:ns * 1024].rearrange("p (s k n) -> p s k n", s=ns, k=8)
    for s_ in range(ns):
        for kc in range(8):
            cp("dve", screp[:, s_, kc, :], scT[:, kc, s_:s_ + 1].broadcast_to([128, 128]))
    mT = sb("mT", [128, DEPTH, 48, ns], F32)
    abT = sb("abT", [128, DEPTH, 48], F32)
    ngT = sb("ngT", [128, 2, DEPTH, 8], F32)
    abrow = sb("abrow", [1, 512], F32)
    abrow_b = sb("abrow_b", [1, 512], BF)
    grow = sb("grow", [1, 512], F32)
    for l in layers:
        dma("sp", abT[:, l, :], ada_bT[l])
    for w_ in range(2):
        for l in layers:
            dma("sp", ngT[:, w_, l, :], normgT[w_, l])
    for l in layers:
        wv = ada_w[l].rearrange("(kc p) n -> p kc n", p=128)
        for jj in range(12):
            w = wslot()
            wj = w[:, :].rearrange("p (kc n) -> p kc n", kc=8)
            dma("pool", wj, wv[:, :, jj * 512:(jj + 1) * 512])
            for q4 in range(4):
                cidx = jj * 4 + q4
                ps = nextps()
                for kc in range(8):
                    mm(ps[:, 0:ns], wj[:, kc, q4 * 128:(q4 + 1) * 128], scT[:, kc, :], kc == 0, kc == 7)
                ts("dve", mT[:, l, cidx, :], ps[:, 0:ns], abT[:, l, cidx:cidx + 1], None, ALU.add)
            if jj in (4, 5, 10, 11):
                which = 0 if jj < 6 else 1
                half = jj % 2
                dma("sp", abrow[:, :], ada_b[l:l + 1, jj * 512:(jj + 1) * 512])
                cp("dve", abrow_b[:, :], abrow[:, :])
                for s_ in range(ns):
                    ps = nextps()
                    for kc in range(8):
                        mm(ps[:, :], screp[:, s_, kc, :], wj[:, kc, :], kc == 0, False)
                    mm(ps[:, :], ones_b[0:1, :], abrow_b[:, :], False, True)
                    cp("act", grow[:, :], ps[0:1, :])
                    dma("sp", gsc[l, which, s_:s_ + 1, half * 512:(half + 1) * 512], grow[:, :])

    if stop_after == "phase0":
        l0 = layers[0]
        for which in range(2):
            for s_ in range(ns):
                r = which * ns + s_
                t_ = xt[0]
                dma("sp", t_[0:1, :], gsc[l0, which, s_:s_ + 1, :])
                dma("sp", out[0, r:r + 1, :], t_[0:1, :])
        P.finalize(["out"])
        stack.close()
        return nc, P.n_ops

    def load_mods(l, s, set_i, which):
        base = which * 24
        A = ABt[:, set_i, 0, :]
        B = ABt[:, set_i, 1, :]
        cp("dve", B, mT[:, l, base:base + 8, s])
        stt(A, mT[:, l, base + 8:base + 16, s], 1.0, ngT[:, which, l, :], ALU.add, ALU.mult)
        dma("sp", Gt[set_i][:, :], gsc[l, which, s:s + 1, :].partition_broadcast(128).rearrange("p o n -> p (o n)"))

    def hsrc(l, b, st):
        if l == layers[0]:
            if st < 2:
                return ctx[b, st * 128:(st + 1) * 128, :]
            return x[b, (st - 2) * 128:(st - 1) * 128, :]
        return hbuf[b, st * 128:(st + 1) * 128, :]

    def normmod_T(l, b, st, src_ap, dstT, col0, sm_i):
        set_i = 0 if st < 2 else 1
        t0 = tmpf[0]
        ab = abf[sm_i % 2]
        ss = small[:, (sm_i % 8) * 2:(sm_i % 8) * 2 + 1]
        rs = small[:, (sm_i % 8) * 2 + 1:(sm_i % 8) * 2 + 2]
        act(t0[:, :], src_ap, AF.Square, accum=ss)
        act(rs, ss, AF.Sqrt, bias=EPS, scale=1.0 / D)
        recip(rs, rs)
        ts("dve", ab[:, :], src_ap, rs, None, ALU.mult)
        ps = nextps()
        psb = ps[:, :].bitcast(BF).rearrange("p (c t) -> p c t", c=8)
        for c in range(8):
            tr(psb[:, c, :], ab[:, c * 128:(c + 1) * 128], ident_b[:, :])
        for c in range(8):
            A = ABt[:, set_i, 0, c:c + 1]
            B = ABt[:, set_i, 1, c:c + 1]
            import os as _os
            _ev = _os.environ.get("PROBE_EVAC", "mixed")
            if _ev == "act" or (_ev == "mixed" and st % 2 == 0):
                act(dstT[:, c, col0:col0 + 128], psb[:, c, :], AF.Identity, bias=B, scale=A)
            else:
                ts("dve", dstT[:, c, col0:col0 + 128], psb[:, c, :], A, B, ALU.mult, ALU.add)

    TILES = [(0, 256)] + [(256 + i * 512, 512) for i in range(4)]

    def da_layer(l, b, j, need_ctx):
        lam_init = 0.8 - 0.6 * math.exp(-0.3 * l)
        attnT = ARB[:, :].rearrange("p (c t) -> p c t", c=8)
        qT = ARC[:, 0:N]
        kT = ARC[:, N:2 * N]
        Vh = ARC[:, 2 * N:3 * N].rearrange("p (s e) -> p s e", s=NST)
        gq = small[:, 32:33]
        gk = small[:, 33:34]
        gs = small[:, 34:35]
        nlam = small[:, 35:36]
        lamrow = small[0:1, 40:44]
        for dst, src in ((gq, da_q_norm_g), (gk, da_k_norm_g)):
            for hh in range(2):
                dma("sp", dst[hh * 64:(hh + 1) * 64, :], src[j:j + 1, :].rearrange("o n -> n o"))
        dma("sp", gs, da_subln_g[j:j + 1, :].rearrange("o n -> n o"))
        ts("dve", gs, gs, 1.0 - lam_init, None, ALU.mult)
        lt = wk[0][0:1, 0:256]
        dma("sp", lt, da_lambda[j:j + 1, :, :].rearrange("o a n -> o (a n)"))
        lp = wk[1][0:1, 0:128]
        tt("dve", lp[:, 0:64], lt[:, 0:64], lt[:, 64:128], ALU.mult)
        tt("dve", lp[:, 64:128], lt[:, 128:192], lt[:, 192:256], ALU.mult)
        P.add("dve", lambda e: e.tensor_reduce(lamrow[:, 0:2], lp.rearrange("o (a n) -> o a n", a=2), AX.X, ALU.add),
              [lp], [lamrow[:, 0:2]])
        act(lamrow[:, 0:2], lamrow[:, 0:2], AF.Exp)
        tt("dve", lamrow[:, 2:3], lamrow[:, 1:2], lamrow[:, 0:1], ALU.subtract)
        ts("dve", lamrow[:, 2:3], lamrow[:, 2:3], -lam_init, None, ALU.add)
        ps = nextps()
        mm(ps[:, 0:1], ones1[0:1, :], lamrow[:, 2:3], True, True)
        cp("dve", nlam, ps[:, 0:1])

        wqkv = da_w_qkv[j].rearrange("(kc p) (s h e) -> p kc s h e", p=128, s=3, h=8)
        for h in range(8):
            w = wslot()
            wh = w[:, 0:3072].rearrange("p (kc s e) -> p kc s e", kc=8, s=3)
            for s3 in range(3):
                dma("pool", wh[:, :, s3, :], wqkv[:, :, s3, h, :])
            for which, dstT, g in ((0, qT, gq), (1, kT, gk)):
                for (t0, tn) in TILES:
                    if not need_ctx and which == 0 and t0 == 0:
                        continue
                    ps = nextps()
                    for kc in range(8):
                        mm(ps[:, 0:tn], wh[:, kc, which, :], aT[:, kc, t0:t0 + tn], kc == 0, kc == 7)
                    sq = nwk()
                    act(sq[:, 0:tn], ps[:, 0:tn], AF.Square)
                    ps2 = nextps()
                    mm(ps2[:, 0:tn], blk64, sq[:, 0:tn], True, True)
                    rstd = nwk()
                    act(rstd[:, 0:tn], ps2[:, 0:tn], AF.Sqrt, bias=EPS)
                    recip(rstd[:, 0:tn], rstd[:, 0:tn])
                    if t0 == 0:
                        stt(dstT[:, t0:t0 + tn], ps[:, 0:tn], g, rstd[:, 0:tn], ALU.mult, ALU.mult)
                    else:
                        qn = nwkb()
                        stt(qn[:, 0:tn], ps[:, 0:tn], g, rstd[:, 0:tn], ALU.mult, ALU.mult)
                        ps3 = nextps()
                        mm(ps3[:, 0:tn], rot_b[:, :], qn[:, 0:tn], True, True)
                        cs_ = rope_tile(0, t0 - L, tn)
                        sn_ = rope_tile(1, t0 - L, tn)
                        tt("pool", cs_, qn[:, 0:tn], cs_, ALU.mult)
                        tt("dve", sn_, ps3[:, 0:tn], sn_, ALU.mult)
                        tt("pool", dstT[:, t0:t0 + tn], cs_, sn_, ALU.add)
            for g4 in range(0, NST, 4):
                nsub = min(4, NST - g4)
                ps = nextps()
                for s_ in range(nsub):
                    st = g4 + s_
                    for kc in range(8):
                        mm(ps[:, s_ * 128:(s_ + 1) * 128], aT[:, kc, st * 128:(st + 1) * 128], wh[:, kc, 2, :], kc == 0, kc == 7)
                cp("act", Vh[:, g4:g4 + nsub, :], ps[:, 0:nsub * 128].rearrange("p (s e) -> p s e", s=nsub))
            for (t0, tn) in TILES:
                if t0 == 0:
                    if not need_ctx:
                        continue
                    nk = 2
                else:
                    nk = NST
                O = [nextps(), nextps()]
                Z = [nextps(), nextps()]
                free = [p_ for p_ in PS if p_ is not O[0] and p_ is not O[1] and p_ is not Z[0] and p_ is not Z[1]]
                steps = [(m, ks) for m in range(2) for ks in range(nk)]

                def s_mm(i):
                    m, ks = steps[i]
                    mm(free[i % 4][:, 0:tn], kT[64 * m:64 * m + 64, ks * 128:(ks + 1) * 128], qT[64 * m:64 * m + 64, t0:t0 + tn], True, True)

                s_mm(0)
                if len(steps) > 1:
                    s_mm(1)
                for i, (m, ks) in enumerate(steps):
                    pt = nwkb()
                    act(pt[:, 0:tn], free[i % 4][:, 0:tn], AF.Exp, scale=0.125)
                    if i + 2 < len(steps):
                        s_mm(i + 2)
                    mm(O[m][:, 0:tn], Vh[:, ks, :], pt[:, 0:tn], ks == 0, ks == nk - 1)
                    mm(Z[m][:, 0:tn], ones_b[:, :], pt[:, 0:tn], ks == 0, ks == nk - 1)
                r0 = nwk()
                r1 = nwk()
                recip(r0[:, 0:tn], Z[0][:, 0:tn])
                recip(r1[:, 0:tn], Z[1][:, 0:tn])
                tt("dve", r0[:, 0:tn], O[0][:, 0:tn], r0[:, 0:tn], ALU.mult)
                tt("dve", r1[:, 0:tn], O[1][:, 0:tn], r1[:, 0:tn], ALU.mult)
                o = nwk()
                stt(o[:, 0:tn], r1[:, 0:tn], nlam, r0[:, 0:tn], ALU.mult, ALU.add)
                sq = nwk()
                act(sq[:, 0:tn], o[:, 0:tn], AF.Square)
                ps2 = free[0]
                mm(ps2[:, 0:tn], ones128, sq[:, 0:tn], True, True)
                rstd = nwk()
                act(rstd[:, 0:tn], ps2[:, 0:tn], AF.Sqrt, bias=EPS)
                recip(rstd[:, 0:tn], rstd[:, 0:tn])
                stt(attnT[:, h, t0:t0 + tn], o[:, 0:tn], gs, rstd[:, 0:tn], ALU.mult, ALU.mult)
        wo_v = da_w_o[j].rearrange("(h p) n -> p h n", p=128)
        wo = []
        for half in range(2):
            w = wslot()
            wv_ = w[:, :].rearrange("p (h n) -> p h n", h=8)
            dma("pool", wv_, wo_v[:, :, half * 512:(half + 1) * 512])
            wo.append(wv_)
        for st in range(NST):
            if st < 2 and not need_ctx:
                continue
            set_i = 0 if st < 2 else 1
            G = Gt[set_i]
            xo = xt[xt_rr[0] % 3]
            xt_rr[0] += 1
            dma("sp", xo[:, :], hsrc(l, b, st))
            for half in range(2):
                ps = nextps()
                for hh in range(8):
                    mm(ps[:, :], attnT[:, hh, st * 128:(st + 1) * 128], wo[half][:, hh, :], hh == 0, hh == 7)
                t = nwk()
                tt("dve", t[:, :], ps[:, :], G[:, half * 512:(half + 1) * 512], ALU.mult)
                tt("pool", xo[:, half * 512:(half + 1) * 512], xo[:, half * 512:(half + 1) * 512], t[:, :], ALU.add)
            dma("sp", hbuf[b, st * 128:(st + 1) * 128, :], xo[:, :])

    def rt_layer(l, b, j, need_ctx):
        dec = sb_rt["dec"]
        qT = ARC[:, 0:2 * N].rearrange("p (c t) -> p c t", c=2)
        kT = ARC[:, 2 * N:4 * N].rearrange("p (c t) -> p c t", c=2)
        qd = ARC[:, 4 * N:6 * N].rearrange("p (c t) -> p c t", c=2)
        Vh = ARB[:, 0:4 * N].rearrange("p (s e) -> p s e", s=NST)
        kd = ARB[:, 4 * N:6 * N].rearrange("p (s e) -> p s e", s=NST)
        Sst = sb_rt["S"]
        Sbf = sb_rt["Sbf"]
        maskT = sb_rt["maskT"]
        dq = sb_rt["dq"]
        dk = sb_rt["dk"]
        dc = sb_rt["dc"]
        oT = sb_rt["oT"]
        otok = tmpf[0][:, :].bitcast(BF)
        dma("sp", dec[:, :], rt_decay[j:j + 1, :, :].rearrange("o a h -> o (a h)").partition_broadcast(128).rearrange("p o n -> p (o n)"))
        act(dec[:, :], dec[:, :], AF.Exp)
        act(dec[:, :], dec[:, :], AF.Ln, bias=1.0, scale=-1.0)
        win = rt_w_in[j].rearrange("(kc p) n -> p kc n", p=128)
        for h in range(4):
            for d_ in range(2):
                lg = dec[:, d_ * 4 + h:d_ * 4 + h + 1]
                rel = nwk()
                ge = nwk()
                ts("dve", rel[:, 0:128], relT, 1.0 if d_ == 0 else -1.0, None, ALU.mult)
                ts("dve", ge[:, 0:128], rel[:, 0:128], 1.0, 0.0, ALU.add, ALU.max)
                ts("dve", ge[:, 0:128], ge[:, 0:128], 1.0, 1.0 / 16.0, ALU.min, ALU.mult)
                ts("dve", rel[:, 0:128], rel[:, 0:128], 0.0, None, ALU.max)
                act(rel[:, 0:128], rel[:, 0:128], AF.Exp, scale=lg)
                tt("dve", maskT[:, d_, :], rel[:, 0:128], ge[:, 0:128], ALU.mult)
                pr = nwk()
                ts("dve", pr[:, 0:128], relT, pos, None, ALU.add)
                if d_ == 0:
                    ts("dve", pr[:, 0:128], pr[:, 0:128], 1.0, None, ALU.add)
                else:
                    ts("dve", pr[:, 0:128], pr[:, 0:128], -1.0, 128.0, ALU.mult, ALU.add)
                act(dq[:, d_, :], pr[:, 0:128], AF.Exp, scale=lg)
                act(dk[:, d_:d_ + 1], rpos if d_ == 0 else pos, AF.Exp, scale=lg)
                ts("dve", dk[:, d_:d_ + 1], dk[:, d_:d_ + 1], 1.0 / 16.0, None, ALU.mult)
                ts("dve", dc[:, d_:d_ + 1], lg, 128.0, None, ALU.mult)
                act(dc[:, d_:d_ + 1], dc[:, d_:d_ + 1], AF.Exp)
            for which, dstT in ((0, qT), (1, kT)):
                wq = []
                for cc in range(2):
                    w = wslot()
                    wv_ = w[:, 0:1024].rearrange("p (kc n) -> p kc n", kc=8)
                    c0 = which * 1024 + h * 256 + cc * 128
                    dma("pool", wv_, win[:, :, c0:c0 + 128])
                    wq.append(wv_)
                for (t0, tn) in TILES:
                    pp = [nextps(), nextps()]
                    for cc in range(2):
                        for kc in range(8):
                            mm(pp[cc][:, 0:tn], wq[cc][:, kc, :], aT[:, kc, t0:t0 + tn], kc == 0, kc == 7)
                    if t0 == 0:
                        cp("act", dstT[:, 0, t0:t0 + tn], pp[0][:, 0:tn])
                        cp("dve", dstT[:, 1, t0:t0 + tn], pp[1][:, 0:tn])
                    else:
                        cs = rope_tile(2, t0 - L, tn)
                        sn = rope_tile(3, t0 - L, tn)
                        a1 = nwk(); b1 = nwk()
                        tt("dve", a1[:, 0:tn], pp[0][:, 0:tn], cs, ALU.mult)
                        tt("dve", b1[:, 0:tn], pp[1][:, 0:tn], sn, ALU.mult)
                        tt("pool", dstT[:, 0, t0:t0 + tn], a1[:, 0:tn], b1[:, 0:tn], ALU.subtract)
                        a2 = nwk(); b2 = nwk()
                        tt("dve", a2[:, 0:tn], pp[0][:, 0:tn], sn, ALU.mult)
                        tt("dve", b2[:, 0:tn], pp[1][:, 0:tn], cs, ALU.mult)
                        tt("pool", dstT[:, 1, t0:t0 + tn], a2[:, 0:tn], b2[:, 0:tn], ALU.add)
            w = wslot()
            wv_ = w[:, :].rearrange("p (kc n) -> p kc n", kc=8)
            dma("pool", wv_, win[:, :, 2048 + h * 512:2048 + (h + 1) * 512])
            for st in range(NST):
                ps = nextps()
                for kc in range(8):
                    mm(ps[:, :], aT[:, kc, st * 128:(st + 1) * 128], wv_[:, kc, :], kc == 0, kc == 7)
                cp("act", Vh[:, st, :], ps[:, :])
            for d_ in range(2):
                w = wslot()
                wg_ = w[:, :].rearrange("p (kc n) -> p kc n", kc=8)
                dma("pool", wg_, win[:, :, 4096 + d_ * 2048 + h * 512:4096 + d_ * 2048 + (h + 1) * 512])
                for cc in range(2):
                    tt("pool" if cc else "dve",
                       qd[:, cc, :].rearrange("p (s t) -> p s t", s=NST),
                       qT[:, cc, :].rearrange("p (s t) -> p s t", s=NST),
                       dq[:, d_:d_ + 1, :].broadcast_to([128, NST, 128]), ALU.mult)
                for st in range(NST):
                    ps = nextps()
                    psb = ps[:, :].bitcast(BF)
                    for cc in range(2):
                        tr(psb[:, cc * 128:(cc + 1) * 128], kT[:, cc, st * 128:(st + 1) * 128], ident_b[:, :])
                    ts("dve", kd[:, st, :], psb[:, 0:256], dk[:, d_:d_ + 1], None, ALU.mult)
                memset("dve", Sst[:, :, :], 0.0)
                memset("pool", Sbf[:, :, :], 0.0)
                order = list(range(NST)) if d_ == 0 else [1, 0] + list(range(NST - 1, 1, -1))
                for st in order:
                    skip_out = (st < 2 and not need_ctx)
                    if not skip_out:
                        pss = nextps()
                        for cc in range(2):
                            mm(pss[:, 0:128], kT[:, cc, st * 128:(st + 1) * 128], qT[:, cc, st * 128:(st + 1) * 128], cc == 0, cc == 1)
                        msk = nwkb()
                        tt("dve", msk[:, 0:128], pss[:, 0:128], maskT[:, d_, :], ALU.mult)
                        py = nextps()
                        mm(py[:, :], msk[:, 0:128], Vh[:, st, :], True, False)
                        for cc in range(2):
                            mm(py[:, :], qd[:, cc, st * 128:(st + 1) * 128], Sbf[:, cc, :], False, cc == 1)
                        pg = nextps()
                        for kc in range(8):
                            mm(pg[:, :], aT[:, kc, st * 128:(st + 1) * 128], wg_[:, kc, :], kc == 0, kc == 7)
                        sg = nwk()
                        act(sg[:, :], pg[:, :], AF.Silu)
                        junk = nwk()
                        ss = small[:, 48 + d_:49 + d_]
                        act(junk[:, :], py[:, :], AF.Square, accum=ss)
                        act(ss, ss, AF.Sqrt, bias=EPS, scale=1.0 / 512.0)
                        recip(ss, ss)
                        ob = nwkb()
                        if d_ == 0:
                            stt(ob[:, :], py[:, :], ss, sg[:, :], ALU.mult, ALU.mult)
                        else:
                            t = nwk()
                            stt(t[:, :], py[:, :], ss, sg[:, :], ALU.mult, ALU.mult)
                            of = nwkb()
                            dma("sp", of[:, :], obuf[st * 128:(st + 1) * 128, h * 512:(h + 1) * 512])
                            tt("pool", ob[:, :], t[:, :], of[:, :], ALU.add)
                        dma("sp", obuf[st * 128:(st + 1) * 128, h * 512:(h + 1) * 512], ob[:, :])
                    for cc in range(2):
                        pu = nextps()
                        mm(pu[:, :], kd[:, st, cc * 128:(cc + 1) * 128], Vh[:, st, :], True, True)
                        stt(Sst[:, cc, :], Sst[:, cc, :], dc[:, d_:d_ + 1], pu[:, :], ALU.mult, ALU.add)
                        cp("act", Sbf[:, cc, :], Sst[:, cc, :])
        ws_pool[0] = WS + WSX
        wo_v = rt_w_o[j].rearrange("(c p) n -> p c n", p=128)
        wo = []
        for half in range(2):
            for c8 in range(2):
                w = wslot()
                wv_ = w[:, :].rearrange("p (c n) -> p c n", c=8)
                dma("pool", wv_, wo_v[:, c8 * 8:(c8 + 1) * 8, half * 512:(half + 1) * 512])
                wo.append(wv_)
        for st in range(NST):
            if st < 2 and not need_ctx:
                continue
            set_i = 0 if st < 2 else 1
            G = Gt[set_i]
            dma("sp", otok, obuf[st * 128:(st + 1) * 128, :])
            for g8 in range(2):
                ps = nextps()
                psb = ps[:, :].bitcast(BF).rearrange("p (c t) -> p c t", c=8)
                for c in range(8):
                    tr(psb[:, c, :], otok[:, (g8 * 8 + c) * 128:(g8 * 8 + c + 1) * 128], ident_b[:, :])
                cp("act", oT[:, g8 * 8:(g8 + 1) * 8, :], psb[:, :, :])
            xo = xt[xt_rr[0] % 3]
            xt_rr[0] += 1
            dma("sp", xo[:, :], hsrc(l, b, st))
            for half in range(2):
                ps = nextps()
                for c in range(16):
                    mm(ps[:, :], oT[:, c, :], wo[half * 2 + c // 8][:, c % 8, :], c == 0, c == 15)
                t = nwk()
                tt("dve", t[:, :], ps[:, :], G[:, half * 512:(half + 1) * 512], ALU.mult)
                tt("pool", xo[:, half * 512:(half + 1) * 512], xo[:, half * 512:(half + 1) * 512], t[:, :], ALU.add)
            dma("sp", hbuf[b, st * 128:(st + 1) * 128, :], xo[:, :])
        ws_pool[0] = WS

    sb_rt = {}
    if any(l % 2 == 1 for l in layers):
        sb_rt["dec"] = sb("rt_dec", [128, 8], F32)
        sb_rt["S"] = sb("rt_S", [128, 2, 512], F32)
        sb_rt["Sbf"] = sb("rt_Sbf", [128, 2, 512], BF)
        sb_rt["maskT"] = sb("rt_maskT", [128, 2, 128], F32)
        sb_rt["dq"] = sb("rt_dq", [128, 2, 128], F32)
        sb_rt["dk"] = sb("rt_dk", [128, 2], F32)
        sb_rt["dc"] = sb("rt_dc", [128, 2], F32)
        sb_rt["oT"] = sb("rt_oT", [128, 16, 128], BF)

    wr = sb("wr", [128, 8, 36], BF)
    wrs = sb("wrs", [128, 8, 36], F32)
    br = sb("br", [128, 36], F32)
    comb = sb("comb", [128, 9, 32], F32)
    rtmp = sb("rtmp", [128, 256], F32)
    fT = ARC[:, 0:8 * 1152].rearrange("p (c t) -> p c t", c=8)
    aTe2 = [ARC[:, 8 * 1152 + i * 2048:8 * 1152 + (i + 1) * 2048].rearrange("p (c t) -> p c t", c=4) for i in range(2)]
    acc = ARB[:, :].bitcast(F32).rearrange("p (s n) -> p s n", s=9)

    def moe_layer(l, b, need_ctx, last):
        ws_pool[0] = WS + WSX
        dma("sp", wrs[:, :, 0:4], moe_w_group[l].rearrange("(kc p) n -> p kc n", p=128))
        dma("sp", wrs[:, :, 4:36], moe_w_expert[l].rearrange("(kc p) n -> p kc n", p=128))
        cp("dve", wr[:, :, :], wrs[:, :, :])
        dma("sp", br[:, 0:4], moe_b_group[l:l + 1, :].partition_broadcast(128).rearrange("p o n -> p (o n)"))
        dma("sp", br[:, 4:36], moe_b_expert[l:l + 1, :].partition_broadcast(128).rearrange("p o n -> p (o n)"))
        wgv = moe_w_gate[l].rearrange("e (kc p) n -> e p kc n", p=128)
        wuv = moe_w_up[l].rearrange("e (kc p) n -> e p kc n", p=128)
        wdv = moe_w_down[l].rearrange("e (kc p) n -> e p kc n", p=128)
        groups = [(0, 9), (9, 9)] if need_ctx else [(2, 8), (10, 8)]
        for (s0, nsub) in groups:
            ntiles = [(i * 384, 384) for i in range(3)] if nsub == 9 else [(0, 512), (512, 512)]
            for s_ in range(nsub):
                st = s0 + s_
                xo = xt[xt_rr[0] % 3]
                xt_rr[0] += 1
                dma("sp", xo[:, :], hbuf[b, st * 128:(st + 1) * 128, :])
                normmod_T(l, b, st, xo[:, :], fT, s_ * 128, s_)
                ps = nextps()
                for kc in range(8):
                    mm(ps[:, 0:36], fT[:, kc, s_ * 128:(s_ + 1) * 128], wr[:, kc, :], kc == 0, kc == 7)
                lg = rtmp[:, 0:36]
                tt("dve", lg, ps[:, 0:36], br[:, :], ALU.add)
                gm = rtmp[:, 40:41]
                P.add("dve", lambda e, lg=lg, gm=gm: e.tensor_reduce(gm, lg[:, 0:4], AX.X, ALU.max), [lg[:, 0:4]], [gm])
                ngm = rtmp[:, 41:42]
                ts("dve", ngm, gm, -1.0, None, ALU.mult)
                ge = rtmp[:, 44:48]
                gsum = rtmp[:, 42:43]
                act(ge, lg[:, 0:4], AF.Exp, bias=ngm, accum=gsum)
                psel = rtmp[:, 43:44]
                recip(psel, gsum)
                gmask = rtmp[:, 48:52]
                tt("dve", gmask, lg[:, 0:4], gm.broadcast_to([128, 4]), ALU.is_ge)
                el = rtmp[:, 64:96]
                tt("dve", el.rearrange("p (g e) -> p g e", g=4), lg[:, 4:36].rearrange("p (g e) -> p g e", g=4),
                   gmask.rearrange("p (g o) -> p g o", o=1).broadcast_to([128, 4, 8]), ALU.mult)
                sel = rtmp[:, 96:104]
                P.add("dve", lambda e, el=el, sel=sel: e.tensor_reduce(sel, el.rearrange("p (g e) -> p e g", g=4), AX.X, ALU.add), [el], [sel])
                top = rtmp[:, 104:112]
                P.add("dve", lambda e, top=top, sel=sel: e.max(top, sel), [sel], [top])
                m1 = rtmp[:, 112:120]
                m2 = rtmp[:, 120:128]
                tt("dve", m1, sel, top[:, 0:1].broadcast_to([128, 8]), ALU.is_ge)
                tt("dve", m2, sel, top[:, 1:2].broadcast_to([128, 8]), ALU.is_ge)
                tt("dve", m2, m2, m1, ALU.subtract)
                dlt = rtmp[:, 128:129]
                tt("dve", dlt, top[:, 1:2], top[:, 0:1], ALU.subtract)
                act(dlt, dlt, AF.Exp)
                w1 = rtmp[:, 129:130]
                w2 = rtmp[:, 130:131]
                ts("dve", w1, dlt, 1.0, None, ALU.add)
                recip(w1, w1)
                tt("dve", w2, dlt, w1, ALU.mult)
                tt("dve", w1, w1, psel, ALU.mult)
                tt("dve", w2, w2, psel, ALU.mult)
                c8 = rtmp[:, 136:144]
                ts("dve", c8, m1, w1, None, ALU.mult)
                stt(c8, m2, w2, c8, ALU.mult, ALU.add)
                tt("dve", comb[:, s_, :].rearrange("p (g e) -> p g e", g=4),
                   gmask.rearrange("p (g o) -> p g o", o=1).broadcast_to([128, 4, 8]),
                   c8.rearrange("p (o e) -> p o e", o=1).broadcast_to([128, 4, 8]), ALU.mult)
            def load_w(e_):
                w1_ = wslot(); w2_ = wslot(); w3_ = wslot()
                wg_ = w1_[:, :].rearrange("p (kc n) -> p kc n", kc=8)
                wu_ = w2_[:, :].rearrange("p (kc n) -> p kc n", kc=8)
                wd_ = w3_[:, :].rearrange("p (kc n) -> p kc n", kc=4)
                dma("pool", wg_, wgv[e_])
                dma("pool", wu_, wuv[e_])
                dma("pool", wd_, wdv[e_])
                return wg_, wu_, wd_

            def gate_up(e_, wts, t0, tn, buf):
                wg_, wu_, wd_ = wts
                for hc in range(4):
                    pg = nextps()
                    pu = nextps()
                    for kc in range(8):
                        mm(pg[:, 0:tn], wg_[:, kc, hc * 128:(hc + 1) * 128], fT[:, kc, t0:t0 + tn], kc == 0, kc == 7)
                    for kc in range(8):
                        mm(pu[:, 0:tn], wu_[:, kc, hc * 128:(hc + 1) * 128], fT[:, kc, t0:t0 + tn], kc == 0, kc == 7)
                    sg = nwk()
                    act(sg[:, 0:tn], pg[:, 0:tn], AF.Silu)
                    tt("dve", buf[:, hc, 0:tn], pu[:, 0:tn], sg[:, 0:tn], ALU.mult)

            def down(e_, wts, t0, tn, buf):
                wd_ = wts[2]
                for sl in range(tn // 128):
                    s_ = t0 // 128 + sl
                    for half in range(2):
                        pd = nextps()
                        for hc in range(4):
                            mm(pd[:, :], buf[:, hc, sl * 128:(sl + 1) * 128], wd_[:, hc, half * 512:(half + 1) * 512], hc == 0, hc == 3)
                        a_ = acc[:, s_, half * 512:(half + 1) * 512]
                        if e_ == 0:
                            ts("dve", a_, pd[:, :], comb[:, s_, e_:e_ + 1], None, ALU.mult)
                        else:
                            stt(a_, pd[:, :], comb[:, s_, e_:e_ + 1], a_, ALU.mult, ALU.add)

            wts_all = {0: load_w(0)}
            pend = None
            k_ = 0
            for e_ in range(NE):
                if e_ + 1 < NE:
                    wts_all[e_ + 1] = load_w(e_ + 1)
                for (t0, tn) in ntiles:
                    buf = aTe2[k_ % 2]
                    k_ += 1
                    gate_up(e_, wts_all[e_], t0, tn, buf)
                    if pend is not None:
                        down(*pend)
                    pend = (e_, wts_all[e_], t0, tn, buf)
            down(*pend)
            for s_ in range(nsub):
                st = s0 + s_
                set_i = 0 if st < 2 else 1
                G = Gt[set_i]
                xo = xt[xt_rr[0] % 3]
                xt_rr[0] += 1
                dma("sp", xo[:, :], hbuf[b, st * 128:(st + 1) * 128, :])
                tt("pool", acc[:, s_, :], acc[:, s_, :], G[:, :], ALU.mult)
                tt("dve", xo[:, :], xo[:, :], acc[:, s_, :], ALU.add)
                if last and st < 2:
                    pass
                elif last:
                    dma("sp", out[b, (st - 2) * 128:(st - 1) * 128, :], xo[:, :])
                else:
                    dma("sp", hbuf[b, st * 128:(st + 1) * 128, :], xo[:, :])
        ws_pool[0] = WS

    for li, l in enumerate(layers):
        need_ctx = l < DEPTH - 1
        last = li == len(layers) - 1
        for b in range(nb):
            load_mods(l, nb, 0, 0)
            load_mods(l, b, 1, 0)
            if stop_after in ("evac_act", "evac_dve"):
                import os as _os
                _pst = int(_os.environ.get("PROBE_ST", 2))
                _pset = 0 if _pst < 2 else 1
                xo = xt[0]
                dma("sp", xo[:, :], hsrc(l, b, _pst))
                ss = small[:, 0:1]; rs = small[:, 1:2]
                act(tmpf[0][:, :], xo[:, :], AF.Square, accum=ss)
                act(rs, ss, AF.Sqrt, bias=EPS, scale=1.0 / D)
                recip(rs, rs)
                ab = abf[0]
                ts("dve", ab[:, :], xo[:, :], rs, None, ALU.mult)
                ps = nextps()
                psb = ps[:, :].bitcast(BF).rearrange("p (c t) -> p c t", c=8)
                for c in range(8):
                    tr(psb[:, c, :], ab[:, c * 128:(c + 1) * 128], ident_b[:, :])
                for c in range(8):
                    A = ABt[:, _pset, 0, c:c + 1]; B = ABt[:, _pset, 1, c:c + 1]
                    if stop_after == "evac_act":
                        act(aT[:, c, 0:128], psb[:, c, :], AF.Identity, bias=B, scale=A)
                    else:
                        ts("dve", aT[:, c, 0:128], psb[:, c, :], A, B, ALU.mult, ALU.add)
                xo2 = xt[1]
                for c in range(8):
                    cp("dve", xo2[:, c * 128:(c + 1) * 128], aT[:, c, 0:128])
                dma("sp", out[0, 0:128, :], xo2[:, :])
                P.finalize(["out"])
                stack.close()
                return nc, P.n_ops
            if stop_after == "tr_only":
                xo = xt[0]
                dma("sp", xo[:, :], hsrc(l, b, 2))
                ab = abf[0]
                cp("dve", ab[:, :], xo[:, :])
                ps = nextps()
                psb = ps[:, :].bitcast(BF).rearrange("p (c t) -> p c t", c=8)
                for c in range(8):
                    tr(psb[:, c, :], ab[:, c * 128:(c + 1) * 128], ident_b[:, :])
                xo2 = xt[1]
                for c in range(8):
                    cp("dve", xo2[:, c * 128:(c + 1) * 128], psb[:, c, :])
                dma("sp", out[0, 0:128, :], xo2[:, :])
                P.finalize(["out"])
                stack.close()
                return nc, P.n_ops
            if stop_after == "norm_only":
                for k_, st in enumerate((0, 2)):
                    xo = xt[k_]
                    dma("sp", xo[:, :], hsrc(l, b, st))
                    ss = small[:, 2 * k_:2 * k_ + 1]
                    rs = small[:, 2 * k_ + 1:2 * k_ + 2]
                    act(tmpf[0][:, :], xo[:, :], AF.Square, accum=ss)
                    act(rs, ss, AF.Sqrt, bias=EPS, scale=1.0 / D)
                    recip(rs, rs)
                    ts("dve", xo[:, :], xo[:, :], rs, None, ALU.mult)
                    dma("sp", out[0, k_ * 128:(k_ + 1) * 128, :], xo[:, :])
                P.finalize(["out"])
                stack.close()
                return nc, P.n_ops
            if stop_after == "mods_only":
                dma("sp", out[0, 0:128, 0:32], ABt[:, :, :, :].rearrange("p a b c -> p (a b c)"))
                dma("sp", out[0, 128:256, :], Gt[0][:, :])
                dma("sp", out[0, 256:384, :], Gt[1][:, :])
                P.finalize(["out"])
                stack.close()
                return nc, P.n_ops
            import os as _os
            _nsub = int(_os.environ.get("PROBE_NSUB", NST))
            for st in range(_nsub):
                xo = xt[xt_rr[0] % 3]
                xt_rr[0] += 1
                dma("sp", xo[:, :], hsrc(l, b, st))
                normmod_T(l, b, st, xo[:, :], aT, st * 128, st)
            if stop_after == "mixer_in2":
                _w = min(1024, _nsub * 128)
                for c in range(8):
                    xo2 = xt[c % 3]
                    cp("dve", xo2[:, 0:_w], aT[:, c, 0:_w])
                    dma("sp", out[0, c * 128:(c + 1) * 128, 0:_w], xo2[:, 0:_w])
                P.finalize(["out"])
                stack.close()
                return nc, P.n_ops
            if stop_after == "mixer_in":
                for c in range(8):
                    dma("pool", out[0, c * 128:(c + 1) * 128, :], aT[:, c, 0:1024])
                P.finalize(["out"])
                stack.close()
                return nc, P.n_ops
            if l % 2 == 0:
                da_layer(l, b, l // 2, need_ctx)
            else:
                rt_layer(l, b, l // 2, need_ctx)
            load_mods(l, nb, 0, 1)
            load_mods(l, b, 1, 1)
            moe_layer(l, b, need_ctx, last)

    P.finalize(["out"])
    stack.close()
    return nc, P.n_ops


def consts_offsets():
    names = [("ident", 128), ("rot", 128), ("blk64", 128), ("ones128", 128), ("ones1", 128), ("pos", 1), ("rpos", 1),
             ("relT", 128)]
    off = {}
    o = 0
    for n, w in names:
        off[n] = o
        o += w
    off["_w"] = o
    return off


def make_consts():
    CO = consts_offsets()
    c = np.zeros((128, CO["_w"]), np.float32)
    c[:, CO["ident"]:CO["ident"] + 128] = np.eye(128, dtype=np.float32)
    R = np.zeros((128, 128), np.float32)
    for m in range(128):
        dd = m % 64
        if dd < 32:
            R[m + 32, m] = -1.0
        else:
            R[m - 32, m] = 1.0
    c[:, CO["rot"]:CO["rot"] + 128] = R
    blk = np.zeros((128, 128), np.float32)
    blk[0:64, 0:64] = 1.0 / 64
    blk[64:128, 64:128] = 1.0 / 64
    c[:, CO["blk64"]:CO["blk64"] + 128] = blk
    c[:, CO["ones128"]:CO["ones128"] + 128] = 1.0 / 128
    c[:, CO["ones1"]:CO["ones1"] + 128] = 1.0
    p = np.arange(128, dtype=np.float32)
    c[:, CO["pos"]] = p
    c[:, CO["rpos"]] = 127.0 - p
    c[:, CO["relT"]:CO["relT"] + 128] = p[None, :] - p[:, None]
    t = np.arange(S)
    row = (t // 64).astype(np.float32)
    col = (t % 64).astype(np.float32)

    def ang(head_dim):
        nf = head_dim // 4
        inv = (10000.0 ** (-np.arange(nf, dtype=np.float32) / nf)).astype(np.float32)
        return np.concatenate([row[:, None] * inv, col[:, None] * inv], axis=-1).astype(np.float32)

    a_da = ang(64)
    pidx = (np.arange(128) % 64) % 32
    rope = np.zeros((4, 128, S), np.float32)
    rope[0] = np.cos(a_da)[:, pidx].T
    rope[1] = np.sin(a_da)[:, pidx].T
    a_rt = ang(256)
    rope[2] = np.cos(a_rt).T
    rope[3] = np.sin(a_rt).T
    return c, rope


_CACHE = {}


def run_cores(inputs, nb, layers, n_cores, batch_ids, stop_after=None, build_only=False):
    key = (nb, tuple(layers), stop_after)
    consts, rope = make_consts()
    if key not in _CACHE:
        _CACHE[key] = build_program(nb, layers, list(consts.shape), stop_after)
    nc, nops = _CACHE[key]
    if build_only:
        return nops
    f = lambda k: np.ascontiguousarray(np.asarray(inputs[k], dtype=np.float32))
    shared = {k: f(k) for k in ["norm1_g", "norm2_g", "ada_w", "ada_b", "da_w_qkv", "da_q_norm_g", "da_k_norm_g",
                                "da_lambda", "da_subln_g", "da_w_o", "rt_w_in", "rt_decay", "rt_w_o", "moe_w_group",
                                "moe_b_group", "moe_w_expert", "moe_b_expert", "moe_w_gate", "moe_w_up", "moe_w_down"]}
    x = f("x"); ctx = f("ctx"); c = f("c"); c_ctx = f("c_ctx")
    ada_bT = np.ascontiguousarray(shared["ada_b"].reshape(DEPTH, 48, 128).transpose(0, 2, 1))
    normgT = np.ascontiguousarray(np.stack([shared["norm1_g"], shared["norm2_g"]]).reshape(2, DEPTH, 8, 128).transpose(0, 1, 3, 2))
    in_maps = []
    for ci in range(n_cores):
        ids = batch_ids[ci]
        call = np.concatenate([c[ids], c_ctx[None, :]], axis=0)
        cT = np.ascontiguousarray(call.reshape(nb + 1, 8, 128).transpose(2, 1, 0))
        m = dict(shared)
        m.update({"x": np.ascontiguousarray(x[ids]), "ctx": np.ascontiguousarray(ctx[ids]), "cT": cT, "consts": consts, "rope": rope,
                  "ada_bT": ada_bT, "normgT": normgT})
        in_maps.append(m)
    res = run_bass_kernel_spmd(nc, in_maps, core_ids=list(range(n_cores)))
    return [r["out"] for r in res.results]


def kernel(**inputs):
    nb = 4
    ids = [list(range(ci * nb, (ci + 1) * nb)) for ci in range(8)]
    outs = run_cores(inputs, nb, [0, 1, 2, 3], 8, ids)
    return np.concatenate(outs, axis=0).astype(np.float32)
```

```python
import math
import numpy as np
import concourse.bass as bass
import concourse.mybir as mybir
from concourse.bass_utils import run_bass_kernel_spmd

F32 = mybir.dt.float32
BF = mybir.dt.bfloat16
I32 = mybir.dt.int32
AF = mybir.ActivationFunctionType
ALU = mybir.AluOpType
AX = mybir.AxisListType

D = 1024
S = 2048
L = 256
N = S + L
NST = N // 128
DEPTH = 4
NE = 32
HID = 512
EPS = 1e-6


def _esize(dt):
    return 2 if dt == BF else 4


class Prog:
    def __init__(self, nc, readonly):
        self.nc = nc
        self.ops = []
        self.readonly = set(readonly)
        self.state = {}

    def add(self, eng, fn, ins=(), outs=(), dma=False):
        self.ops.append([eng, fn, list(ins), list(outs), dma])

    def _grans(self, ap):
        name = ap.tensor.name
        if name in self.readonly:
            return name, ()
        es = _esize(ap.dtype)
        dims = [list(d) for d in ap.ap]
        sp = str(ap.space) if hasattr(ap, "space") else ""
        onchip = ("SB" in sp.upper()) or ("PSUM" in sp.upper()) or ("STATE" in sp.upper())
        if name.startswith("ps"):
            return name, (0,)
        if onchip:
            pstride = dims[0][0]
            off = ap.offset % pstride if pstride else ap.offset
            free = dims[1:]
            G = 512
        else:
            off = ap.offset
            free = dims
            G = 65536
        free = [d for d in free if d[1] > 1]
        if not free:
            lo = off * es
            return name, tuple(range(lo // G, (lo + es - 1) // G + 1))
        inner = free[-1]
        outer = free[:-1]
        nint = 1
        for d in outer:
            nint *= d[1]
        span_in = (inner[1] - 1) * abs(inner[0]) + 1
        res = set()
        if nint > 128:
            hi = off + sum((d[1] - 1) * abs(d[0]) for d in free) + 1
            return name, tuple(range(off * es // G, (hi * es - 1) // G + 1))
        idx = [0] * len(outer)
        while True:
            st = off + sum(i * d[0] for i, d in zip(idx, outer))
            res.update(range(st * es // G, ((st + span_in) * es - 1) // G + 1))
            k = len(outer) - 1
            while k >= 0:
                idx[k] += 1
                if idx[k] < outer[k][1]:
                    break
                idx[k] = 0
                k -= 1
            if k < 0:
                break
        return name, tuple(res)

    def finalize(self, out_names):
        nc = self.nc
        ops = self.ops
        n = len(ops)
        deps = [None] * n
        needed = [False] * n
        state = {}
        for i, (eng, fn, ins, outs, dma) in enumerate(ops):
            dset = set()
            rg = [self._grans(a) for a in ins]
            wg = [self._grans(a) for a in outs]
            for name, gs in rg:
                for g in gs:
                    st = state.get((name, g))
                    if st is not None and st[0] is not None:
                        dset.add(st[0])
            for name, gs in wg:
                for g in gs:
                    st = state.get((name, g))
                    if st is not None:
                        if st[0] is not None:
                            dset.add(st[0])
                        dset.update(st[1].values())
            rkey = eng if (not dma and eng in ("pe", "act", "dve")) else i
            for name, gs in rg:
                for g in gs:
                    st = state.get((name, g))
                    if st is None:
                        state[(name, g)] = [None, {rkey: i}]
                    else:
                        st[1][rkey] = i
            for name, gs in wg:
                for g in gs:
                    state[(name, g)] = [i, {}]
            dset.discard(i)
            dl = []
            for d in dset:
                if ops[d][0] == "pe" and eng == "pe" and not ops[d][4] and not dma:
                    continue
                dl.append(d)
                needed[d] = True
            deps[i] = dl
        streams = ["pe", "act", "dve", "pool", "sp"]
        NDS = 8
        import contextlib
        self._stack = contextlib.ExitStack()
        mk = lambda nm: self._stack.enter_context(nc.semaphore(nm))
        esem = {s: [mk(f"e_{s}_0")] for s in streams}
        ecount = {s: 0 for s in streams}
        dsem = {s: [mk(f"d_{s}_{k}") for k in range(NDS)] for s in ("sp", "pool", "act")}
        dcount = {s: [0] * NDS for s in dsem}
        dnext = {s: 0 for s in dsem}
        signal = [None] * n
        prewait = [None] * n
        for i, (eng, fn, ins, outs, dma) in enumerate(ops):
            if dma:
                k = dnext[eng]
                dnext[eng] = (k + 1) % NDS
                if dcount[eng][k] > 0:
                    prewait[i] = (dsem[eng][k], dcount[eng][k])
                dcount[eng][k] += 16
                signal[i] = (dsem[eng][k], dcount[eng][k], 16)
            elif needed[i]:
                if ecount[eng] >= 30000:
                    esem[eng].append(mk(f"e_{eng}_{len(esem[eng])}"))
                    ecount[eng] = 0
                ecount[eng] += 1
                signal[i] = (esem[eng][-1], ecount[eng], 1)
        known = {s: {} for s in streams}
        waits = [None] * n
        for i, (eng, fn, ins, outs, dma) in enumerate(ops):
            w = {}
            kn = known[eng]
            if prewait[i] is not None:
                sem, val = prewait[i]
                if kn.get(sem.num, 0) < val:
                    w[sem.num] = (sem, val)
            for d in deps[i]:
                sem, val, _ = signal[d]
                if kn.get(sem.num, 0) < val and w.get(sem.num, (None, 0))[1] < val:
                    w[sem.num] = (sem, val)
            for k, (sem, val) in w.items():
                kn[k] = val
            waits[i] = list(w.values())
        final = []
        for s in dsem:
            for k in range(NDS):
                if dcount[s][k] > 0:
                    final.append((dsem[s][k], dcount[s][k]))
        by = {s: [i for i in range(n) if ops[i][0] == s] for s in streams}
        self.n_ops = n
        finalcount = {}
        for i in range(n):
            if signal[i] is not None:
                sem, val, _ = signal[i]
                finalcount[sem.num] = max(finalcount.get(sem.num, 0), val)
        bad = []
        for i in range(n):
            for sem, val in waits[i]:
                if val > finalcount.get(sem.num, 0):
                    bad.append((i, ops[i][0], sem.num, val, finalcount.get(sem.num, 0)))
        for i in range(n):
            for d in deps[i]:
                if signal[d] is None:
                    bad.append((i, ops[i][0], "dep-without-signal", d, ops[d][0]))
        nd = sum(1 for o in ops if o[4])
        print(f"[prog] ops={n} dma={nd} waits={sum(len(w) for w in waits)} "
              f"signals={sum(1 for x in signal if x is not None)} unreachable={len(bad)}", flush=True)
        if bad:
            raise RuntimeError(f"unreachable semaphore waits (first 10): {bad[:10]}")

        def emit(stream, e):
            for i in by[stream]:
                for sem, val in waits[i]:
                    e.wait_ge(sem, val)
                ins = ops[i][1](e)
                if signal[i] is not None:
                    ins.then_inc(signal[i][0], signal[i][2])
            if stream == "sp":
                for sem, val in final:
                    e.wait_ge(sem, val)

        with nc.Block() as block:
            @block.tensor
            def _(e):
                emit("pe", e)

            @block.scalar
            def _(e):
                emit("act", e)

            @block.vector
            def _(e):
                emit("dve", e)

            @block.gpsimd
            def _(e):
                emit("pool", e)

            @block.sync
            def _(e):
                emit("sp", e)
        self._stack.close()


def build_program(nb, layers, consts_shape, stop_after=None):
    nc = bass.Bass("TRN2", target_bir_lowering=False)
    dt = nc.dram_tensor
    ext = {}

    def EI(name, shape, dtype=F32):
        ext[name] = dt(name, list(shape), dtype, kind="ExternalInput").ap()
        return ext[name]

    x = EI("x", [nb, S, D])
    ctx = EI("ctx", [nb, L, D])
    cT = EI("cT", [128, 8, nb + 1])
    consts = EI("consts", consts_shape)
    rope = EI("rope", [4, 128, S])
    ada_bT = EI("ada_bT", [DEPTH, 128, 48])
    normgT = EI("normgT", [2, DEPTH, 128, 8])
    norm1_g = EI("norm1_g", [DEPTH, D])
    norm2_g = EI("norm2_g", [DEPTH, D])
    ada_w = EI("ada_w", [DEPTH, D, 6 * D])
    ada_b = EI("ada_b", [DEPTH, 6 * D])
    da_w_qkv = EI("da_w_qkv", [2, D, 3 * D])
    da_q_norm_g = EI("da_q_norm_g", [2, 64])
    da_k_norm_g = EI("da_k_norm_g", [2, 64])
    da_lambda = EI("da_lambda", [2, 4, 64])
    da_subln_g = EI("da_subln_g", [2, 128])
    da_w_o = EI("da_w_o", [2, D, D])
    rt_w_in = EI("rt_w_in", [2, D, 8192])
    rt_decay = EI("rt_decay", [2, 2, 4])
    rt_w_o = EI("rt_w_o", [2, 2048, D])
    moe_w_group = EI("moe_w_group", [DEPTH, D, 4])
    moe_b_group = EI("moe_b_group", [DEPTH, 4])
    moe_w_expert = EI("moe_w_expert", [DEPTH, D, 32])
    moe_b_expert = EI("moe_b_expert", [DEPTH, 32])
    moe_w_gate = EI("moe_w_gate", [DEPTH, NE, D, HID])
    moe_w_up = EI("moe_w_up", [DEPTH, NE, D, HID])
    moe_w_down = EI("moe_w_down", [DEPTH, NE, HID, D])
    out = dt("out", [nb, S, D], F32, kind="ExternalOutput").ap()
    hbuf = dt("hbuf", [nb, N, D], F32, kind="Internal").ap()
    gsc = dt("gsc", [DEPTH, 2, nb + 1, D], F32, kind="Internal").ap()
    obuf = dt("obuf", [N, 2048], BF, kind="Internal").ap()

    P = Prog(nc, readonly=list(ext.keys()))
    import contextlib
    stack = contextlib.ExitStack()

    def sb(name, shape, dtype):
        return stack.enter_context(nc.sbuf_tensor(name, list(shape), dtype))

    def psum(name):
        return stack.enter_context(nc.psum_tensor(name, [128, 512], F32))

    PS = [psum(f"ps{i}") for i in range(8)]
    ps_rr = [0]

    def dma(q, out_ap, in_ap):
        P.add(q, lambda e: e.dma_start(out=out_ap, in_=in_ap), [in_ap], [out_ap], dma=True)

    def mm(out_ap, lhsT, rhs, start, stop):
        P.add("pe", lambda e: e.matmul(out_ap, lhsT, rhs, start=start, stop=stop), [lhsT, rhs], [out_ap])

    def tr(out_ap, in_ap, ident):
        P.add("pe", lambda e: e.transpose(out_ap, in_ap, ident), [in_ap, ident], [out_ap])

    def act(out_ap, in_ap, func, bias=None, scale=None, accum=None):
        kw = {}
        ins = [in_ap]
        outs = [out_ap]
        if bias is not None:
            kw["bias"] = bias
            if not isinstance(bias, float):
                ins.append(bias)
        if scale is not None:
            kw["scale"] = scale
            if not isinstance(scale, float):
                ins.append(scale)
        if accum is not None:
            kw["accum_out"] = accum
            outs.append(accum)
        P.add("act", lambda e: e.activation(out_ap, in_ap, func, **kw), ins, outs)

    def tt(eng, out_ap, a, b, op):
        P.add(eng, lambda e: e.tensor_tensor(out_ap, a, b, op), [a, b], [out_ap])

    def ts(eng, out_ap, a, s1, s2, op0, op1=None):
        ins = [a] + [s for s in (s1, s2) if s is not None and not isinstance(s, (float, int))]
        if op1 is None:
            P.add(eng, lambda e: e.tensor_scalar(out_ap, a, s1, None, op0), ins, [out_ap])
        else:
            P.add(eng, lambda e: e.tensor_scalar(out_ap, a, s1, s2, op0, op1), ins, [out_ap])

    def stt(out_ap, a, s, b, op0, op1):
        ins = [a, b] + ([] if isinstance(s, (float, int)) else [s])
        P.add("dve", lambda e: e.scalar_tensor_tensor(out_ap, a, s, b, op0, op1), ins, [out_ap])

    def cp(eng, out_ap, in_ap):
        if eng == "act":
            P.add("act", lambda e: e.copy(out_ap, in_ap), [in_ap], [out_ap])
        else:
            P.add(eng, lambda e: e.tensor_copy(out_ap, in_ap), [in_ap], [out_ap])

    def recip(out_ap, in_ap):
        P.add("dve", lambda e: e.reciprocal(out_ap, in_ap), [in_ap], [out_ap])

    def memset(eng, ap, v):
        P.add(eng, lambda e: e.memset(ap, v), [], [ap])

    def nextps():
        p = PS[ps_rr[0] % 8]
        ps_rr[0] += 1
        return p

    CO = consts_offsets()
    cst = sb("cst", [128, CO["_w"]], F32)
    dma("sp", cst[:, :], consts[:, :])
    ident_b = sb("ident_b", [128, 128], BF)
    rot_b = sb("rot_b", [128, 128], BF)
    ones_b = sb("ones_b", [128, 128], BF)

    def C(name, w):
        o = CO[name]
        return cst[:, o:o + w]

    cp("dve", ident_b[:, :], C("ident", 128))
    cp("dve", rot_b[:, :], C("rot", 128))
    memset("dve", ones_b[:, :], 1.0)
    def rope_tile(idx, c0, tn):
        w = nwk()
        dma("sp", w[:, 0:tn], rope[idx, :, c0:c0 + tn])
        return w[:, 0:tn]
    blk64 = C("blk64", 128)
    ones128 = C("ones128", 128)
    ones1 = C("ones1", 128)
    pos = C("pos", 1)
    rpos = C("rpos", 1)
    relT = C("relT", 128)
    ident_f = C("ident", 128)

    ARA = sb("ARA", [128, 8 * N], BF)
    ARB = sb("ARB", [128, 8 * N], BF)
    ARC = sb("ARC", [128, 6 * N], BF)
    WS = [sb(f"WS{i}", [128, 4096], BF) for i in range(3)]
    WSX = [ARA[:, i * 4096:(i + 1) * 4096] for i in range(4)]
    ws_rr = [0]
    ws_pool = [WS]

    def wslot():
        pool = ws_pool[0]
        w = pool[ws_rr[0] % len(pool)]
        ws_rr[0] += 1
        return w

    aT = ARA[:, :].rearrange("p (c t) -> p c t", c=8)
    xt = [sb(f"xt{i}", [128, D], F32) for i in range(3)]
    xt_rr = [0]
    tmpf = [sb(f"tmpf{i}", [128, D], F32) for i in range(1)]
    abf = [sb(f"abf{i}", [128, D], BF) for i in range(2)]
    small = sb("small", [128, 64], F32)
    Gt = [sb(f"Gt{s}", [128, D], F32) for s in range(2)]
    ABt = sb("ABt", [128, 2, 2, 8], F32)
    wk = [sb(f"wk{i}", [128, 512], F32) for i in range(8)]
    wk_rr = [0]
    wkb = [sb(f"wkb{i}", [128, 512], BF) for i in range(4)]
    wkb_rr = [0]

    def nwk():
        w = wk[wk_rr[0] % len(wk)]
        wk_rr[0] += 1
        return w

    def nwkb():
        w = wkb[wkb_rr[0] % len(wkb)]
        wkb_rr[0] += 1
        return w

    ns = nb + 1
    scT = sb("scT", [128, 8, ns], BF)
    cTs = sb("cTs", [128, 8, ns], F32)
    dma("sp", cTs[:, :, :], cT[:, :, :])
    act(scT[:, :, :], cTs[:, :, :], AF.Silu)
    screp = ARB[:, 0:ns * 1024].rearrange("p (s k n) -> p s k n", s=ns, k=8)
    for s_ in range(ns):
        for kc in range(8):
            cp("dve", screp[:, s_, kc, :], scT[:, kc, s_:s_ + 1].broadcast_to([128, 128]))
    mT = sb("mT", [128, DEPTH, 48, ns], F32)
    abT = sb("abT", [128, DEPTH, 48], F32)
    ngT = sb("ngT", [128, 2, DEPTH, 8], F32)
    abrow = sb("abrow", [1, 512], F32)
    abrow_b = sb("abrow_b", [1, 512], BF)
    grow = sb("grow", [1, 512], F32)
    for l in layers:
        dma("sp", abT[:, l, :], ada_bT[l])
    for w_ in range(2):
        for l in layers:
            dma("sp", ngT[:, w_, l, :], normgT[w_, l])
    for l in layers:
        wv = ada_w[l].rearrange("(kc p) n -> p kc n", p=128)
        for jj in range(12):
            w = wslot()
            wj = w[:, :].rearrange("p (kc n) -> p kc n", kc=8)
            dma("pool", wj, wv[:, :, jj * 512:(jj + 1) * 512])
            for q4 in range(4):
                cidx = jj * 4 + q4
                ps = nextps()
                for kc in range(8):
                    mm(ps[:, 0:ns], wj[:, kc, q4 * 128:(q4 + 1) * 128], scT[:, kc, :], kc == 0, kc == 7)
                ts("dve", mT[:, l, cidx, :], ps[:, 0:ns], abT[:, l, cidx:cidx + 1], None, ALU.add)
            if jj in (4, 5, 10, 11):
                which = 0 if jj < 6 else 1
                half = jj % 2
                dma("sp", abrow[:, :], ada_b[l:l + 1, jj * 512:(jj + 1) * 512])
                cp("dve", abrow_b[:, :], abrow[:, :])
                for s_ in range(ns):
                    ps = nextps()
                    for kc in range(8):
                        mm(ps[:, :], screp[:, s_, kc, :], wj[:, kc, :], kc == 0, False)
                    mm(ps[:, :], ones_b[0:1, :], abrow_b[:, :], False, True)
                    cp("act", grow[:, :], ps[0:1, :])
                    dma("sp", gsc[l, which, s_:s_ + 1, half * 512:(half + 1) * 512], grow[:, :])

    if stop_after == "phase0":
        l0 = layers[0]
        for which in range(2):
            for s_ in range(ns):
                r = which * ns + s_
                t_ = xt[0]
                dma("sp", t_[0:1, :], gsc[l0, which, s_:s_ + 1, :])
                dma("sp", out[0, r:r + 1, :], t_[0:1, :])
        P.finalize(["out"])
        stack.close()
        return nc, P.n_ops

    def load_mods(l, s, set_i, which):
        base = which * 24
        A = ABt[:, set_i, 0, :]
        B = ABt[:, set_i, 1, :]
        cp("dve", B, mT[:, l, base:base + 8, s])
        stt(A, mT[:, l, base + 8:base + 16, s], 1.0, ngT[:, which, l, :], ALU.add, ALU.mult)
        dma("sp", Gt[set_i][:, :], gsc[l, which, s:s + 1, :].partition_broadcast(128).rearrange("p o n -> p (o n)"))

    def hsrc(l, b, st):
        if l == layers[0]:
            if st < 2:
                return ctx[b, st * 128:(st + 1) * 128, :]
            return x[b, (st - 2) * 128:(st - 1) * 128, :]
        return hbuf[b, st * 128:(st + 1) * 128, :]

    def normmod_T(l, b, st, src_ap, dstT, col0, sm_i):
        set_i = 0 if st < 2 else 1
        t0 = tmpf[0]
        ab = abf[sm_i % 2]
        ss = small[:, (sm_i % 8) * 2:(sm_i % 8) * 2 + 1]
        rs = small[:, (sm_i % 8) * 2 + 1:(sm_i % 8) * 2 + 2]
        act(t0[:, :], src_ap, AF.Square, accum=ss)
        act(rs, ss, AF.Sqrt, bias=EPS, scale=1.0 / D)
        recip(rs, rs)
        ts("dve", ab[:, :], src_ap, rs, None, ALU.mult)
        ps = nextps()
        psb = ps[:, :].bitcast(BF).rearrange("p (c t) -> p c t", c=8)
        for c in range(8):
            tr(psb[:, c, :], ab[:, c * 128:(c + 1) * 128], ident_b[:, :])
        for c in range(8):
            A = ABt[:, set_i, 0, c:c + 1]
            B = ABt[:, set_i, 1, c:c + 1]
            import os as _os
            _ev = _os.environ.get("PROBE_EVAC", "mixed")
            if _ev == "act" or (_ev == "mixed" and st % 2 == 0):
                act(dstT[:, c, col0:col0 + 128], psb[:, c, :], AF.Identity, bias=B, scale=A)
            else:
                ts("dve", dstT[:, c, col0:col0 + 128], psb[:, c, :], A, B, ALU.mult, ALU.add)

    TILES = [(0, 256)] + [(256 + i * 512, 512) for i in range(4)]

    def da_layer(l, b, j, need_ctx):
        lam_init = 0.8 - 0.6 * math.exp(-0.3 * l)
        attnT = ARB[:, :].rearrange("p (c t) -> p c t", c=8)
        qT = ARC[:, 0:N]
        kT = ARC[:, N:2 * N]
        Vh = ARC[:, 2 * N:3 * N].rearrange("p (s e) -> p s e", s=NST)
        gq = small[:, 32:33]
        gk = small[:, 33:34]
        gs = small[:, 34:35]
        nlam = small[:, 35:36]
        lamrow = small[0:1, 40:44]
        for dst, src in ((gq, da_q_norm_g), (gk, da_k_norm_g)):
            for hh in range(2):
                dma("sp", dst[hh * 64:(hh + 1) * 64, :], src[j:j + 1, :].rearrange("o n -> n o"))
        dma("sp", gs, da_subln_g[j:j + 1, :].rearrange("o n -> n o"))
        ts("dve", gs, gs, 1.0 - lam_init, None, ALU.mult)
        lt = wk[0][0:1, 0:256]
        dma("sp", lt, da_lambda[j:j + 1, :, :].rearrange("o a n -> o (a n)"))
        lp = wk[1][0:1, 0:128]
        tt("dve", lp[:, 0:64], lt[:, 0:64], lt[:, 64:128], ALU.mult)
        tt("dve", lp[:, 64:128], lt[:, 128:192], lt[:, 192:256], ALU.mult)
        P.add("dve", lambda e: e.tensor_reduce(lamrow[:, 0:2], lp.rearrange("o (a n) -> o a n", a=2), AX.X, ALU.add),
              [lp], [lamrow[:, 0:2]])
        act(lamrow[:, 0:2], lamrow[:, 0:2], AF.Exp)
        tt("dve", lamrow[:, 2:3], lamrow[:, 1:2], lamrow[:, 0:1], ALU.subtract)
        ts("dve", lamrow[:, 2:3], lamrow[:, 2:3], -lam_init, None, ALU.add)
        ps = nextps()
        mm(ps[:, 0:1], ones1[0:1, :], lamrow[:, 2:3], True, True)
        cp("dve", nlam, ps[:, 0:1])

        wqkv = da_w_qkv[j].rearrange("(kc p) (s h e) -> p kc s h e", p=128, s=3, h=8)
        for h in range(8):
            w = wslot()
            wh = w[:, 0:3072].rearrange("p (kc s e) -> p kc s e", kc=8, s=3)
            for s3 in range(3):
                dma("pool", wh[:, :, s3, :], wqkv[:, :, s3, h, :])
            for which, dstT, g in ((0, qT, gq), (1, kT, gk)):
                for (t0, tn) in TILES:
                    if not need_ctx and which == 0 and t0 == 0:
                        continue
                    ps = nextps()
                    for kc in range(8):
                        mm(ps[:, 0:tn], wh[:, kc, which, :], aT[:, kc, t0:t0 + tn], kc == 0, kc == 7)
                    sq = nwk()
                    act(sq[:, 0:tn], ps[:, 0:tn], AF.Square)
                    ps2 = nextps()
                    mm(ps2[:, 0:tn], blk64, sq[:, 0:tn], True, True)
                    rstd = nwk()
                    act(rstd[:, 0:tn], ps2[:, 0:tn], AF.Sqrt, bias=EPS)
                    recip(rstd[:, 0:tn], rstd[:, 0:tn])
                    if t0 == 0:
                        stt(dstT[:, t0:t0 + tn], ps[:, 0:tn], g, rstd[:, 0:tn], ALU.mult, ALU.mult)
                    else:
                        qn = nwkb()
                        stt(qn[:, 0:tn], ps[:, 0:tn], g, rstd[:, 0:tn], ALU.mult, ALU.mult)
                        ps3 = nextps()
                        mm(ps3[:, 0:tn], rot_b[:, :], qn[:, 0:tn], True, True)
                        cs_ = rope_tile(0, t0 - L, tn)
                        sn_ = rope_tile(1, t0 - L, tn)
                        tt("pool", cs_, qn[:, 0:tn], cs_, ALU.mult)
                        tt("dve", sn_, ps3[:, 0:tn], sn_, ALU.mult)
                        tt("pool", dstT[:, t0:t0 + tn], cs_, sn_, ALU.add)
            for g4 in range(0, NST, 4):
                nsub = min(4, NST - g4)
                ps = nextps()
                for s_ in range(nsub):
                    st = g4 + s_
                    for kc in range(8):
                        mm(ps[:, s_ * 128:(s_ + 1) * 128], aT[:, kc, st * 128:(st + 1) * 128], wh[:, kc, 2, :], kc == 0, kc == 7)
                cp("act", Vh[:, g4:g4 + nsub, :], ps[:, 0:nsub * 128].rearrange("p (s e) -> p s e", s=nsub))
            for (t0, tn) in TILES:
                if t0 == 0:
                    if not need_ctx:
                        continue
                    nk = 2
                else:
                    nk = NST
                O = [nextps(), nextps()]
                Z = [nextps(), nextps()]
                free = [p_ for p_ in PS if p_ is not O[0] and p_ is not O[1] and p_ is not Z[0] and p_ is not Z[1]]
                steps = [(m, ks) for m in range(2) for ks in range(nk)]

                def s_mm(i):
                    m, ks = steps[i]
                    mm(free[i % 4][:, 0:tn], kT[64 * m:64 * m + 64, ks * 128:(ks + 1) * 128], qT[64 * m:64 * m + 64, t0:t0 + tn], True, True)

                s_mm(0)
                if len(steps) > 1:
                    s_mm(1)
                for i, (m, ks) in enumerate(steps):
                    pt = nwkb()
                    act(pt[:, 0:tn], free[i % 4][:, 0:tn], AF.Exp, scale=0.125)
                    if i + 2 < len(steps):
                        s_mm(i + 2)
                    mm(O[m][:, 0:tn], Vh[:, ks, :], pt[:, 0:tn], ks == 0, ks == nk - 1)
                    mm(Z[m][:, 0:tn], ones_b[:, :], pt[:, 0:tn], ks == 0, ks == nk - 1)
                r0 = nwk()
                r1 = nwk()
                recip(r0[:, 0:tn], Z[0][:, 0:tn])
                recip(r1[:, 0:tn], Z[1][:, 0:tn])
                tt("dve", r0[:, 0:tn], O[0][:, 0:tn], r0[:, 0:tn], ALU.mult)
                tt("dve", r1[:, 0:tn], O[1][:, 0:tn], r1[:, 0:tn], ALU.mult)
                o = nwk()
                stt(o[:, 0:tn], r1[:, 0:tn], nlam, r0[:, 0:tn], ALU.mult, ALU.add)
                sq = nwk()
                act(sq[:, 0:tn], o[:, 0:tn], AF.Square)
                ps2 = free[0]
                mm(ps2[:, 0:tn], ones128, sq[:, 0:tn], True, True)
                rstd = nwk()
                act(rstd[:, 0:tn], ps2[:, 0:tn], AF.Sqrt, bias=EPS)
                recip(rstd[:, 0:tn], rstd[:, 0:tn])
                stt(attnT[:, h, t0:t0 + tn], o[:, 0:tn], gs, rstd[:, 0:tn], ALU.mult, ALU.mult)
        wo_v = da_w_o[j].rearrange("(h p) n -> p h n", p=128)
        wo = []
        for half in range(2):
            w = wslot()
            wv_ = w[:, :].rearrange("p (h n) -> p h n", h=8)
            dma("pool", wv_, wo_v[:, :, half * 512:(half + 1) * 512])
            wo.append(wv_)
        for st in range(NST):
            if st < 2 and not need_ctx:
                continue
            set_i = 0 if st < 2 else 1
            G = Gt[set_i]
            xo = xt[xt_rr[0] % 3]
            xt_rr[0] += 1
            dma("sp", xo[:, :], hsrc(l, b, st))
            for half in range(2):
                ps = nextps()
                for hh in range(8):
                    mm(ps[:, :], attnT[:, hh, st * 128:(st + 1) * 128], wo[half][:, hh, :], hh == 0, hh == 7)
                t = nwk()
                tt("dve", t[:, :], ps[:, :], G[:, half * 512:(half + 1) * 512], ALU.mult)
                tt("pool", xo[:, half * 512:(half + 1) * 512], xo[:, half * 512:(half + 1) * 512], t[:, :], ALU.add)
            dma("sp", hbuf[b, st * 128:(st + 1) * 128, :], xo[:, :])

    def rt_layer(l, b, j, need_ctx):
        dec = sb_rt["dec"]
        qT = ARC[:, 0:2 * N].rearrange("p (c t) -> p c t", c=2)
        kT = ARC[:, 2 * N:4 * N].rearrange("p (c t) -> p c t", c=2)
        qd = ARC[:, 4 * N:6 * N].rearrange("p (c t) -> p c t", c=2)
        Vh = ARB[:, 0:4 * N].rearrange("p (s e) -> p s e", s=NST)
        kd = ARB[:, 4 * N:6 * N].rearrange("p (s e) -> p s e", s=NST)
        Sst = sb_rt["S"]
        Sbf = sb_rt["Sbf"]
        maskT = sb_rt["maskT"]
        dq = sb_rt["dq"]
        dk = sb_rt["dk"]
        dc = sb_rt["dc"]
        oT = sb_rt["oT"]
        otok = tmpf[0][:, :].bitcast(BF)
        dma("sp", dec[:, :], rt_decay[j:j + 1, :, :].rearrange("o a h -> o (a h)").partition_broadcast(128).rearrange("p o n -> p (o n)"))
        act(dec[:, :], dec[:, :], AF.Exp)
        act(dec[:, :], dec[:, :], AF.Ln, bias=1.0, scale=-1.0)
        win = rt_w_in[j].rearrange("(kc p) n -> p kc n", p=128)
        for h in range(4):
            for d_ in range(2):
                lg = dec[:, d_ * 4 + h:d_ * 4 + h + 1]
                rel = nwk()
                ge = nwk()
                ts("dve", rel[:, 0:128], relT, 1.0 if d_ == 0 else -1.0, None, ALU.mult)
                ts("dve", ge[:, 0:128], rel[:, 0:128], 1.0, 0.0, ALU.add, ALU.max)
                ts("dve", ge[:, 0:128], ge[:, 0:128], 1.0, 1.0 / 16.0, ALU.min, ALU.mult)
                ts("dve", rel[:, 0:128], rel[:, 0:128], 0.0, None, ALU.max)
                act(rel[:, 0:128], rel[:, 0:128], AF.Exp, scale=lg)
                tt("dve", maskT[:, d_, :], rel[:, 0:128], ge[:, 0:128], ALU.mult)
                pr = nwk()
                ts("dve", pr[:, 0:128], relT, pos, None, ALU.add)
                if d_ == 0:
                    ts("dve", pr[:, 0:128], pr[:, 0:128], 1.0, None, ALU.add)
                else:
                    ts("dve", pr[:, 0:128], pr[:, 0:128], -1.0, 128.0, ALU.mult, ALU.add)
                act(dq[:, d_, :], pr[:, 0:128], AF.Exp, scale=lg)
                act(dk[:, d_:d_ + 1], rpos if d_ == 0 else pos, AF.Exp, scale=lg)
                ts("dve", dk[:, d_:d_ + 1], dk[:, d_:d_ + 1], 1.0 / 16.0, None, ALU.mult)
                ts("dve", dc[:, d_:d_ + 1], lg, 128.0, None, ALU.mult)
                act(dc[:, d_:d_ + 1], dc[:, d_:d_ + 1], AF.Exp)
            for which, dstT in ((0, qT), (1, kT)):
                wq = []
                for cc in range(2):
                    w = wslot()
                    wv_ = w[:, 0:1024].rearrange("p (kc n) -> p kc n", kc=8)
                    c0 = which * 1024 + h * 256 + cc * 128
                    dma("pool", wv_, win[:, :, c0:c0 + 128])
                    wq.append(wv_)
                for (t0, tn) in TILES:
                    pp = [nextps(), nextps()]
                    for cc in range(2):
                        for kc in range(8):
                            mm(pp[cc][:, 0:tn], wq[cc][:, kc, :], aT[:, kc, t0:t0 + tn], kc == 0, kc == 7)
                    if t0 == 0:
                        cp("act", dstT[:, 0, t0:t0 + tn], pp[0][:, 0:tn])
                        cp("dve", dstT[:, 1, t0:t0 + tn], pp[1][:, 0:tn])
                    else:
                        cs = rope_tile(2, t0 - L, tn)
                        sn = rope_tile(3, t0 - L, tn)
                        a1 = nwk(); b1 = nwk()
                        tt("dve", a1[:, 0:tn], pp[0][:, 0:tn], cs, ALU.mult)
                        tt("dve", b1[:, 0:tn], pp[1][:, 0:tn], sn, ALU.mult)
                        tt("pool", dstT[:, 0, t0:t0 + tn], a1[:, 0:tn], b1[:, 0:tn], ALU.subtract)
                        a2 = nwk(); b2 = nwk()
                        tt("dve", a2[:, 0:tn], pp[0][:, 0:tn], sn, ALU.mult)
                        tt("dve", b2[:, 0:tn], pp[1][:, 0:tn], cs, ALU.mult)
                        tt("pool", dstT[:, 1, t0:t0 + tn], a2[:, 0:tn], b2[:, 0:tn], ALU.add)
            w = wslot()
            wv_ = w[:, :].rearrange("p (kc n) -> p kc n", kc=8)
            dma("pool", wv_, win[:, :, 2048 + h * 512:2048 + (h + 1) * 512])
            for st in range(NST):
                ps = nextps()
                for kc in range(8):
                    mm(ps[:, :], aT[:, kc, st * 128:(st + 1) * 128], wv_[:, kc, :], kc == 0, kc == 7)
                cp("act", Vh[:, st, :], ps[:, :])
            for d_ in range(2):
                w = wslot()
                wg_ = w[:, :].rearrange("p (kc n) -> p kc n", kc=8)
                dma("pool", wg_, win[:, :, 4096 + d_ * 2048 + h * 512:4096 + d_ * 2048 + (h + 1) * 512])
                for cc in range(2):
                    tt("pool" if cc else "dve",
                       qd[:, cc, :].rearrange("p (s t) -> p s t", s=NST),
                       qT[:, cc, :].rearrange("p (s t) -> p s t", s=NST),
                       dq[:, d_:d_ + 1, :].broadcast_to([128, NST, 128]), ALU.mult)
                for st in range(NST):
                    ps = nextps()
                    psb = ps[:, :].bitcast(BF)
                    for cc in range(2):
                        tr(psb[:, cc * 128:(cc + 1) * 128], kT[:, cc, st * 128:(st + 1) * 128], ident_b[:, :])
                    ts("dve", kd[:, st, :], psb[:, 0:256], dk[:, d_:d_ + 1], None, ALU.mult)
                memset("dve", Sst[:, :, :], 0.0)
                memset("pool", Sbf[:, :, :], 0.0)
                order = list(range(NST)) if d_ == 0 else [1, 0] + list(range(NST - 1, 1, -1))
                for st in order:
                    skip_out = (st < 2 and not need_ctx)
                    if not skip_out:
                        pss = nextps()
                        for cc in range(2):
                            mm(pss[:, 0:128], kT[:, cc, st * 128:(st + 1) * 128], qT[:, cc, st * 128:(st + 1) * 128], cc == 0, cc == 1)
                        pg = nextps()
                        for kc in range(8):
                            mm(pg[:, :], aT[:, kc, st * 128:(st + 1) * 128], wg_[:, kc, :], kc == 0, kc == 7)
                    pus = []
                    for cc in range(2):
                        pu = nextps()
                        mm(pu[:, :], kd[:, st, cc * 128:(cc + 1) * 128], Vh[:, st, :], True, True)
                        pus.append(pu)
                    if not skip_out:
                        msk = nwkb()
                        tt("dve", msk[:, 0:128], pss[:, 0:128], maskT[:, d_, :], ALU.mult)
                        py = nextps()
                        mm(py[:, :], msk[:, 0:128], Vh[:, st, :], True, False)
                        for cc in range(2):
                            mm(py[:, :], qd[:, cc, st * 128:(st + 1) * 128], Sbf[:, cc, :], False, cc == 1)
                        sg = nwk()
                        act(sg[:, :], pg[:, :], AF.Silu)
                        junk = nwk()
                        ss = small[:, 48 + d_:49 + d_]
                        act(junk[:, :], py[:, :], AF.Square, accum=ss)
                        act(ss, ss, AF.Sqrt, bias=EPS, scale=1.0 / 512.0)
                        recip(ss, ss)
                        ob = nwkb()
                        if d_ == 0:
                            stt(ob[:, :], py[:, :], ss, sg[:, :], ALU.mult, ALU.mult)
                        else:
                            t = nwk()
                            stt(t[:, :], py[:, :], ss, sg[:, :], ALU.mult, ALU.mult)
                            of = nwkb()
                            dma("sp", of[:, :], obuf[st * 128:(st + 1) * 128, h * 512:(h + 1) * 512])
                            tt("pool", ob[:, :], t[:, :], of[:, :], ALU.add)
                        dma("sp", obuf[st * 128:(st + 1) * 128, h * 512:(h + 1) * 512], ob[:, :])
                    for cc in range(2):
                        stt(Sst[:, cc, :], Sst[:, cc, :], dc[:, d_:d_ + 1], pus[cc][:, :], ALU.mult, ALU.add)
                        cp("act", Sbf[:, cc, :], Sst[:, cc, :])
        ws_pool[0] = WS + WSX
        wo_v = rt_w_o[j].rearrange("(c p) n -> p c n", p=128)
        wo = []
        for half in range(2):
            for c8 in range(2):
                w = wslot()
                wv_ = w[:, :].rearrange("p (c n) -> p c n", c=8)
                dma("pool", wv_, wo_v[:, c8 * 8:(c8 + 1) * 8, half * 512:(half + 1) * 512])
                wo.append(wv_)
        for st in range(NST):
            if st < 2 and not need_ctx:
                continue
            set_i = 0 if st < 2 else 1
            G = Gt[set_i]
            dma("sp", otok, obuf[st * 128:(st + 1) * 128, :])
            for g8 in range(2):
                ps = nextps()
                psb = ps[:, :].bitcast(BF).rearrange("p (c t) -> p c t", c=8)
                for c in range(8):
                    tr(psb[:, c, :], otok[:, (g8 * 8 + c) * 128:(g8 * 8 + c + 1) * 128], ident_b[:, :])
                cp("act", oT[:, g8 * 8:(g8 + 1) * 8, :], psb[:, :, :])
            xo = xt[xt_rr[0] % 3]
            xt_rr[0] += 1
            dma("sp", xo[:, :], hsrc(l, b, st))
            for half in range(2):
                ps = nextps()
                for c in range(16):
                    mm(ps[:, :], oT[:, c, :], wo[half * 2 + c // 8][:, c % 8, :], c == 0, c == 15)
                t = nwk()
                tt("dve", t[:, :], ps[:, :], G[:, half * 512:(half + 1) * 512], ALU.mult)
                tt("pool", xo[:, half * 512:(half + 1) * 512], xo[:, half * 512:(half + 1) * 512], t[:, :], ALU.add)
            dma("sp", hbuf[b, st * 128:(st + 1) * 128, :], xo[:, :])
        ws_pool[0] = WS

    sb_rt = {}
    if any(l % 2 == 1 for l in layers):
        sb_rt["dec"] = sb("rt_dec", [128, 8], F32)
        sb_rt["S"] = sb("rt_S", [128, 2, 512], F32)
        sb_rt["Sbf"] = sb("rt_Sbf", [128, 2, 512], BF)
        sb_rt["maskT"] = sb("rt_maskT", [128, 2, 128], F32)
        sb_rt["dq"] = sb("rt_dq", [128, 2, 128], F32)
        sb_rt["dk"] = sb("rt_dk", [128, 2], F32)
        sb_rt["dc"] = sb("rt_dc", [128, 2], F32)
        sb_rt["oT"] = sb("rt_oT", [128, 16, 128], BF)

    wr = sb("wr", [128, 8, 36], BF)
    wrs = sb("wrs", [128, 8, 36], F32)
    br = sb("br", [128, 36], F32)
    comb = sb("comb", [128, 9, 32], F32)
    rtmp = sb("rtmp", [128, 256], F32)
    fT = ARC[:, 0:8 * 1152].rearrange("p (c t) -> p c t", c=8)
    aTe2 = [ARC[:, 8 * 1152 + i * 2048:8 * 1152 + (i + 1) * 2048].rearrange("p (c t) -> p c t", c=4) for i in range(2)]
    acc = ARB[:, :].bitcast(F32).rearrange("p (s n) -> p s n", s=9)

    def moe_layer(l, b, need_ctx, last):
        ws_pool[0] = WS + WSX
        dma("sp", wrs[:, :, 0:4], moe_w_group[l].rearrange("(kc p) n -> p kc n", p=128))
        dma("sp", wrs[:, :, 4:36], moe_w_expert[l].rearrange("(kc p) n -> p kc n", p=128))
        cp("dve", wr[:, :, :], wrs[:, :, :])
        dma("sp", br[:, 0:4], moe_b_group[l:l + 1, :].partition_broadcast(128).rearrange("p o n -> p (o n)"))
        dma("sp", br[:, 4:36], moe_b_expert[l:l + 1, :].partition_broadcast(128).rearrange("p o n -> p (o n)"))
        wgv = moe_w_gate[l].rearrange("e (kc p) n -> e p kc n", p=128)
        wuv = moe_w_up[l].rearrange("e (kc p) n -> e p kc n", p=128)
        wdv = moe_w_down[l].rearrange("e (kc p) n -> e p kc n", p=128)
        groups = [(0, 9), (9, 9)] if need_ctx else [(2, 8), (10, 8)]
        for (s0, nsub) in groups:
            ntiles = [(i * 384, 384) for i in range(3)] if nsub == 9 else [(0, 512), (512, 512)]
            for s_ in range(nsub):
                st = s0 + s_
                xo = xt[xt_rr[0] % 3]
                xt_rr[0] += 1
                dma("sp", xo[:, :], hbuf[b, st * 128:(st + 1) * 128, :])
                normmod_T(l, b, st, xo[:, :], fT, s_ * 128, s_)
                ps = nextps()
                for kc in range(8):
                    mm(ps[:, 0:36], fT[:, kc, s_ * 128:(s_ + 1) * 128], wr[:, kc, :], kc == 0, kc == 7)
                lg = rtmp[:, 0:36]
                tt("dve", lg, ps[:, 0:36], br[:, :], ALU.add)
                gm = rtmp[:, 40:41]
                P.add("dve", lambda e, lg=lg, gm=gm: e.tensor_reduce(gm, lg[:, 0:4], AX.X, ALU.max), [lg[:, 0:4]], [gm])
                ngm = rtmp[:, 41:42]
                ts("dve", ngm, gm, -1.0, None, ALU.mult)
                ge = rtmp[:, 44:48]
                gsum = rtmp[:, 42:43]
                act(ge, lg[:, 0:4], AF.Exp, bias=ngm, accum=gsum)
                psel = rtmp[:, 43:44]
                recip(psel, gsum)
                gmask = rtmp[:, 48:52]
                tt("dve", gmask, lg[:, 0:4], gm.broadcast_to([128, 4]), ALU.is_ge)
                el = rtmp[:, 64:96]
                tt("dve", el.rearrange("p (g e) -> p g e", g=4), lg[:, 4:36].rearrange("p (g e) -> p g e", g=4),
                   gmask.rearrange("p (g o) -> p g o", o=1).broadcast_to([128, 4, 8]), ALU.mult)
                sel = rtmp[:, 96:104]
                P.add("dve", lambda e, el=el, sel=sel: e.tensor_reduce(sel, el.rearrange("p (g e) -> p e g", g=4), AX.X, ALU.add), [el], [sel])
                top = rtmp[:, 104:112]
                P.add("dve", lambda e, top=top, sel=sel: e.max(top, sel), [sel], [top])
                m1 = rtmp[:, 112:120]
                m2 = rtmp[:, 120:128]
                tt("dve", m1, sel, top[:, 0:1].broadcast_to([128, 8]), ALU.is_ge)
                tt("dve", m2, sel, top[:, 1:2].broadcast_to([128, 8]), ALU.is_ge)
                tt("dve", m2, m2, m1, ALU.subtract)
                dlt = rtmp[:, 128:129]
                tt("dve", dlt, top[:, 1:2], top[:, 0:1], ALU.subtract)
                act(dlt, dlt, AF.Exp)
                w1 = rtmp[:, 129:130]
                w2 = rtmp[:, 130:131]
                ts("dve", w1, dlt, 1.0, None, ALU.add)
                recip(w1, w1)
                tt("dve", w2, dlt, w1, ALU.mult)
                tt("dve", w1, w1, psel, ALU.mult)
                tt("dve", w2, w2, psel, ALU.mult)
                c8 = rtmp[:, 136:144]
                ts("dve", c8, m1, w1, None, ALU.mult)
                stt(c8, m2, w2, c8, ALU.mult, ALU.add)
                tt("dve", comb[:, s_, :].rearrange("p (g e) -> p g e", g=4),
                   gmask.rearrange("p (g o) -> p g o", o=1).broadcast_to([128, 4, 8]),
                   c8.rearrange("p (o e) -> p o e", o=1).broadcast_to([128, 4, 8]), ALU.mult)
            def load_w(e_):
                w1_ = wslot(); w2_ = wslot(); w3_ = wslot()
                wg_ = w1_[:, :].rearrange("p (kc n) -> p kc n", kc=8)
                wu_ = w2_[:, :].rearrange("p (kc n) -> p kc n", kc=8)
                wd_ = w3_[:, :].rearrange("p (kc n) -> p kc n", kc=4)
                dma("pool", wg_, wgv[e_])
                dma("pool", wu_, wuv[e_])
                dma("pool", wd_, wdv[e_])
                return wg_, wu_, wd_

            def gate_up(e_, wts, t0, tn, buf):
                wg_, wu_, wd_ = wts
                for hc in range(4):
                    pg = nextps()
                    pu = nextps()
                    for kc in range(8):
                        mm(pg[:, 0:tn], wg_[:, kc, hc * 128:(hc + 1) * 128], fT[:, kc, t0:t0 + tn], kc == 0, kc == 7)
                    for kc in range(8):
                        mm(pu[:, 0:tn], wu_[:, kc, hc * 128:(hc + 1) * 128], fT[:, kc, t0:t0 + tn], kc == 0, kc == 7)
                    sg = nwk()
                    act(sg[:, 0:tn], pg[:, 0:tn], AF.Silu)
                    tt("dve", buf[:, hc, 0:tn], pu[:, 0:tn], sg[:, 0:tn], ALU.mult)

            def down(e_, wts, t0, tn, buf):
                wd_ = wts[2]
                for sl in range(tn // 128):
                    s_ = t0 // 128 + sl
                    for half in range(2):
                        pd = nextps()
                        for hc in range(4):
                            mm(pd[:, :], buf[:, hc, sl * 128:(sl + 1) * 128], wd_[:, hc, half * 512:(half + 1) * 512], hc == 0, hc == 3)
                        a_ = acc[:, s_, half * 512:(half + 1) * 512]
                        if e_ == 0:
                            ts("dve", a_, pd[:, :], comb[:, s_, e_:e_ + 1], None, ALU.mult)
                        else:
                            stt(a_, pd[:, :], comb[:, s_, e_:e_ + 1], a_, ALU.mult, ALU.add)

            wts_all = {0: load_w(0)}
            pend = None
            k_ = 0
            for e_ in range(NE):
                if e_ + 1 < NE:
                    wts_all[e_ + 1] = load_w(e_ + 1)
                for (t0, tn) in ntiles:
                    buf = aTe2[k_ % 2]
                    k_ += 1
                    gate_up(e_, wts_all[e_], t0, tn, buf)
                    if pend is not None:
                        down(*pend)
                    pend = (e_, wts_all[e_], t0, tn, buf)
            down(*pend)
            for s_ in range(nsub):
                st = s0 + s_
                set_i = 0 if st < 2 else 1
                G = Gt[set_i]
                xo = xt[xt_rr[0] % 3]
                xt_rr[0] += 1
                dma("sp", xo[:, :], hbuf[b, st * 128:(st + 1) * 128, :])
                tt("pool", acc[:, s_, :], acc[:, s_, :], G[:, :], ALU.mult)
                tt("dve", xo[:, :], xo[:, :], acc[:, s_, :], ALU.add)
                if last and st < 2:
                    pass
                elif last:
                    dma("sp", out[b, (st - 2) * 128:(st - 1) * 128, :], xo[:, :])
                else:
                    dma("sp", hbuf[b, st * 128:(st + 1) * 128, :], xo[:, :])
        ws_pool[0] = WS

    for li, l in enumerate(layers):
        need_ctx = l < DEPTH - 1
        last = li == len(layers) - 1
        for b in range(nb):
            load_mods(l, nb, 0, 0)
            load_mods(l, b, 1, 0)
            if stop_after in ("evac_act", "evac_dve"):
                import os as _os
                _pst = int(_os.environ.get("PROBE_ST", 2))
                _pset = 0 if _pst < 2 else 1
                xo = xt[0]
                dma("sp", xo[:, :], hsrc(l, b, _pst))
                ss = small[:, 0:1]; rs = small[:, 1:2]
                act(tmpf[0][:, :], xo[:, :], AF.Square, accum=ss)
                act(rs, ss, AF.Sqrt, bias=EPS, scale=1.0 / D)
                recip(rs, rs)
                ab = abf[0]
                ts("dve", ab[:, :], xo[:, :], rs, None, ALU.mult)
                ps = nextps()
                psb = ps[:, :].bitcast(BF).rearrange("p (c t) -> p c t", c=8)
                for c in range(8):
                    tr(psb[:, c, :], ab[:, c * 128:(c + 1) * 128], ident_b[:, :])
                for c in range(8):
                    A = ABt[:, _pset, 0, c:c + 1]; B = ABt[:, _pset, 1, c:c + 1]
                    if stop_after == "evac_act":
                        act(aT[:, c, 0:128], psb[:, c, :], AF.Identity, bias=B, scale=A)
                    else:
                        ts("dve", aT[:, c, 0:128], psb[:, c, :], A, B, ALU.mult, ALU.add)
                xo2 = xt[1]
                for c in range(8):
                    cp("dve", xo2[:, c * 128:(c + 1) * 128], aT[:, c, 0:128])
                dma("sp", out[0, 0:128, :], xo2[:, :])
                P.finalize(["out"])
                stack.close()
                return nc, P.n_ops
            if stop_after == "tr_only":
                xo = xt[0]
                dma("sp", xo[:, :], hsrc(l, b, 2))
                ab = abf[0]
                cp("dve", ab[:, :], xo[:, :])
                ps = nextps()
                psb = ps[:, :].bitcast(BF).rearrange("p (c t) -> p c t", c=8)
                for c in range(8):
                    tr(psb[:, c, :], ab[:, c * 128:(c + 1) * 128], ident_b[:, :])
                xo2 = xt[1]
                for c in range(8):
                    cp("dve", xo2[:, c * 128:(c + 1) * 128], psb[:, c, :])
                dma("sp", out[0, 0:128, :], xo2[:, :])
                P.finalize(["out"])
                stack.close()
                return nc, P.n_ops
            if stop_after == "norm_only":
                for k_, st in enumerate((0, 2)):
                    xo = xt[k_]
                    dma("sp", xo[:, :], hsrc(l, b, st))
                    ss = small[:, 2 * k_:2 * k_ + 1]
                    rs = small[:, 2 * k_ + 1:2 * k_ + 2]
                    act(tmpf[0][:, :], xo[:, :], AF.Square, accum=ss)
                    act(rs, ss, AF.Sqrt, bias=EPS, scale=1.0 / D)
                    recip(rs, rs)
                    ts("dve", xo[:, :], xo[:, :], rs, None, ALU.mult)
                    dma("sp", out[0, k_ * 128:(k_ + 1) * 128, :], xo[:, :])
                P.finalize(["out"])
                stack.close()
                return nc, P.n_ops
            if stop_after == "mods_only":
                dma("sp", out[0, 0:128, 0:32], ABt[:, :, :, :].rearrange("p a b c -> p (a b c)"))
                dma("sp", out[0, 128:256, :], Gt[0][:, :])
                dma("sp", out[0, 256:384, :], Gt[1][:, :])
                P.finalize(["out"])
                stack.close()
                return nc, P.n_ops
            import os as _os
            _nsub = int(_os.environ.get("PROBE_NSUB", NST))
            for st in range(_nsub):
                xo = xt[xt_rr[0] % 3]
                xt_rr[0] += 1
                dma("sp", xo[:, :], hsrc(l, b, st))
                normmod_T(l, b, st, xo[:, :], aT, st * 128, st)
            if stop_after == "mixer_in2":
                _w = min(1024, _nsub * 128)
                for c in range(8):
                    xo2 = xt[c % 3]
                    cp("dve", xo2[:, 0:_w], aT[:, c, 0:_w])
                    dma("sp", out[0, c * 128:(c + 1) * 128, 0:_w], xo2[:, 0:_w])
                P.finalize(["out"])
                stack.close()
                return nc, P.n_ops
            if stop_after == "mixer_in":
                for c in range(8):
                    dma("pool", out[0, c * 128:(c + 1) * 128, :], aT[:, c, 0:1024])
                P.finalize(["out"])
                stack.close()
                return nc, P.n_ops
            if l % 2 == 0:
                da_layer(l, b, l // 2, need_ctx)
            else:
                rt_layer(l, b, l // 2, need_ctx)
            load_mods(l, nb, 0, 1)
            load_mods(l, b, 1, 1)
            moe_layer(l, b, need_ctx, last)

    P.finalize(["out"])
    stack.close()
    return nc, P.n_ops


def consts_offsets():
    names = [("ident", 128), ("rot", 128), ("blk64", 128), ("ones128", 128), ("ones1", 128), ("pos", 1), ("rpos", 1),
             ("relT", 128)]
    off = {}
    o = 0
    for n, w in names:
        off[n] = o
        o += w
    off["_w"] = o
    return off


def make_consts():
    CO = consts_offsets()
    c = np.zeros((128, CO["_w"]), np.float32)
    c[:, CO["ident"]:CO["ident"] + 128] = np.eye(128, dtype=np.float32)
    R = np.zeros((128, 128), np.float32)
    for m in range(128):
        dd = m % 64
        if dd < 32:
            R[m + 32, m] = -1.0
        else:
            R[m - 32, m] = 1.0
    c[:, CO["rot"]:CO["rot"] + 128] = R
    blk = np.zeros((128, 128), np.float32)
    blk[0:64, 0:64] = 1.0 / 64
    blk[64:128, 64:128] = 1.0 / 64
    c[:, CO["blk64"]:CO["blk64"] + 128] = blk
    c[:, CO["ones128"]:CO["ones128"] + 128] = 1.0 / 128
    c[:, CO["ones1"]:CO["ones1"] + 128] = 1.0
    p = np.arange(128, dtype=np.float32)
    c[:, CO["pos"]] = p
    c[:, CO["rpos"]] = 127.0 - p
    c[:, CO["relT"]:CO["relT"] + 128] = p[None, :] - p[:, None]
    t = np.arange(S)
    row = (t // 64).astype(np.float32)
    col = (t % 64).astype(np.float32)

    def ang(head_dim):
        nf = head_dim // 4
        inv = (10000.0 ** (-np.arange(nf, dtype=np.float32) / nf)).astype(np.float32)
        return np.concatenate([row[:, None] * inv, col[:, None] * inv], axis=-1).astype(np.float32)

    a_da = ang(64)
    pidx = (np.arange(128) % 64) % 32
    rope = np.zeros((4, 128, S), np.float32)
    rope[0] = np.cos(a_da)[:, pidx].T
    rope[1] = np.sin(a_da)[:, pidx].T
    a_rt = ang(256)
    rope[2] = np.cos(a_rt).T
    rope[3] = np.sin(a_rt).T
    return c, rope


_CACHE = {}


def run_cores(inputs, nb, layers, n_cores, batch_ids, stop_after=None, build_only=False):
    key = (nb, tuple(layers), stop_after)
    consts, rope = make_consts()
    if key not in _CACHE:
        _CACHE[key] = build_program(nb, layers, list(consts.shape), stop_after)
    nc, nops = _CACHE[key]
    if build_only:
        return nops
    f = lambda k: np.ascontiguousarray(np.asarray(inputs[k], dtype=np.float32))
    shared = {k: f(k) for k in ["norm1_g", "norm2_g", "ada_w", "ada_b", "da_w_qkv", "da_q_norm_g", "da_k_norm_g",
                                "da_lambda", "da_subln_g", "da_w_o", "rt_w_in", "rt_decay", "rt_w_o", "moe_w_group",
                                "moe_b_group", "moe_w_expert", "moe_b_expert", "moe_w_gate", "moe_w_up", "moe_w_down"]}
    x = f("x"); ctx = f("ctx"); c = f("c"); c_ctx = f("c_ctx")
    ada_bT = np.ascontiguousarray(shared["ada_b"].reshape(DEPTH, 48, 128).transpose(0, 2, 1))
    normgT = np.ascontiguousarray(np.stack([shared["norm1_g"], shared["norm2_g"]]).reshape(2, DEPTH, 8, 128).transpose(0, 1, 3, 2))
    in_maps = []
    for ci in range(n_cores):
        ids = batch_ids[ci]
        call = np.concatenate([c[ids], c_ctx[None, :]], axis=0)
        cT = np.ascontiguousarray(call.reshape(nb + 1, 8, 128).transpose(2, 1, 0))
        m = dict(shared)
        m.update({"x": np.ascontiguousarray(x[ids]), "ctx": np.ascontiguousarray(ctx[ids]), "cT": cT, "consts": consts, "rope": rope,
                  "ada_bT": ada_bT, "normgT": normgT})
        in_maps.append(m)
    res = run_bass_kernel_spmd(nc, in_maps, core_ids=list(range(n_cores)))
    return [r["out"] for r in res.results]


def kernel(**inputs):
    nb = 4
    ids = [list(range(ci * nb, (ci + 1) * nb)) for ci in range(8)]
    outs = run_cores(inputs, nb, [0, 1, 2, 3], 8, ids)
    return np.concatenate(outs, axis=0).astype(np.float32)
```
